# Optimizing a Trainium2 kernel written in Bass

```python
import jax, jax.numpy as jnp
from jax import lax
import numpy as np

D_MODEL = 1024
BATCH = 4
SEQ = 8192
DEPTH = 2

GRID_W = 64
CTX_LEN = 256
N_EVEN = (DEPTH + 1) // 2
N_ODD = DEPTH // 2
HEAD_DIM = 64
N_HEADS = (D_MODEL // 2) // HEAD_DIM
N_KV_HEADS = 2
GROUP = N_HEADS // N_KV_HEADS
WINDOW = 128
ATT_BLOCK = 128
ROPE_THETA = 10000.0
FOURIER_W = D_MODEL // 2
FOURIER_GROUPS = 4
FOURIER_GROUP_W = FOURIER_W // FOURIER_GROUPS
Q_W = N_HEADS * HEAD_DIM
KV_W = N_KV_HEADS * HEAD_DIM
EVEN_MIX_W = FOURIER_W + Q_W
EVEN_IN_W = EVEN_MIX_W + 2 * KV_W
CONV_W = 31
CONV_CH = D_MODEL
N_EXPERTS = 16
N_GROUPS = 4
EXPERTS_PER_GROUP = N_EXPERTS // N_GROUPS
TOP_K = 2
GROUP_SCORE_K = 2
EXPERT_FF = 512
MOE_BLOCK = 128
EPS = 1e-6
NEG_INF = -1e30

kernel_name = 'hybrid_fourier_swa_conformer_grouped_moe'

f32 = jnp.float32


def _rmsnorm(x, g):
    xf = x.astype(f32)
    y = xf * lax.rsqrt(jnp.mean(xf * xf, axis=-1, keepdims=True) + EPS)
    return (y * g.astype(f32)).astype(x.dtype)


def _layernorm(x, g, b):
    xf = x.astype(f32)
    mu = jnp.mean(xf, axis=-1, keepdims=True)
    var = jnp.mean(jnp.square(xf - mu), axis=-1, keepdims=True)
    return ((xf - mu) * lax.rsqrt(var + EPS) * g.astype(f32) + b.astype(f32)).astype(x.dtype)


def _adaln(cond, w, b):
    m = jax.nn.silu(cond) @ w + b
    parts = jnp.split(m, 6, axis=-1)
    if m.ndim == 2:
        parts = [p_[:, None, :] for p_ in parts]
    return parts


def _modulate(xn, shift, scale):
    return xn * (1 + scale) + shift


def _rope_half(xh, pos):
    half = xh.shape[-1] // 2
    inv = ROPE_THETA ** (-jnp.arange(half, dtype=f32) / half)
    ang = pos[:, None] * inv[None, :]
    cos = jnp.cos(ang)[None, :, None, :]
    sin = jnp.sin(ang)[None, :, None, :]
    x1 = xh[..., :half].astype(f32)
    x2 = xh[..., half:].astype(f32)
    return jnp.concatenate([x1 * cos - x2 * sin, x1 * sin + x2 * cos], axis=-1).astype(xh.dtype)


def _axial_rope(x, row, col):
    h = x.shape[-1] // 2
    return jnp.concatenate([_rope_half(x[..., :h], row), _rope_half(x[..., h:], col)], axis=-1)


def _fourier_mix(u):
    B, L, _ = u.shape
    ug = u.reshape(B, L, FOURIER_GROUPS, FOURIER_GROUP_W).astype(f32)
    y = jnp.fft.fftn(ug, axes=(1, 3), norm='ortho').real
    return y.reshape(B, L, FOURIER_W).astype(u.dtype)


def _sink_softmax(logits, sink):
    sk = jnp.broadcast_to(sink, logits.shape[:-1] + (1,))
    p = jax.nn.softmax(jnp.concatenate([logits, sk], axis=-1), axis=-1)
    return p[..., :-1]


def _window_attention(q, k, v, ck, cv, sink):
    B, L, _, _ = q.shape
    C = ck.shape[1]
    nb = L // ATT_BLOCK
    scale = HEAD_DIM ** -0.5
    qg = q.reshape(B, nb, ATT_BLOCK, N_KV_HEADS, GROUP, HEAD_DIM).transpose(1, 0, 2, 3, 4, 5)
    pad = ((0, 0), (ATT_BLOCK, ATT_BLOCK), (0, 0), (0, 0))
    kp = jnp.pad(k, pad)
    vp = jnp.pad(v, pad)
    idx = jnp.arange(nb)[:, None] * ATT_BLOCK + jnp.arange(3 * ATT_BLOCK)[None, :]
    kb = jnp.moveaxis(kp[:, idx], 1, 0)
    vb = jnp.moveaxis(vp[:, idx], 1, 0)
    sink_l = sink.astype(f32).reshape(N_KV_HEADS, GROUP)[None, :, :, None, None]
    offs = jnp.arange(3 * ATT_BLOCK)[None, :] - ATT_BLOCK - jnp.arange(ATT_BLOCK)[:, None]

    def block(args):
        qb, kbb, vbb, j = args
        kpos = j * ATT_BLOCK - ATT_BLOCK + jnp.arange(3 * ATT_BLOCK)
        valid = (jnp.abs(offs) <= WINDOW) & ((kpos >= 0) & (kpos < L))[None, :]
        s_c = jnp.einsum('bqkgd,bckd->bkgqc', qb, ck, preferred_element_type=f32) * scale
        s_l = jnp.einsum('bqkgd,bpkd->bkgqp', qb, kbb, preferred_element_type=f32) * scale
        s_l = jnp.where(valid, s_l, NEG_INF)
        p = _sink_softmax(jnp.concatenate([s_c, s_l], axis=-1), sink_l)
        p_c = p[..., :C].astype(vbb.dtype)
        p_l = p[..., C:].astype(vbb.dtype)
        return (jnp.einsum('bkgqc,bckd->bqkgd', p_c, cv)
                + jnp.einsum('bkgqp,bpkd->bqkgd', p_l, vbb))

    o = lax.map(block, (qg, kb, vb, jnp.arange(nb)))
    return o.transpose(1, 0, 2, 3, 4, 5).reshape(B, L, Q_W)


def _context_attention(qc, ck, cv, sink):
    B, C, _, _ = qc.shape
    qg = qc.reshape(B, C, N_KV_HEADS, GROUP, HEAD_DIM)
    s = jnp.einsum('bqkgd,bckd->bkgqc', qg, ck, preferred_element_type=f32) * (HEAD_DIM ** -0.5)
    p = _sink_softmax(s, sink.astype(f32).reshape(N_KV_HEADS, GROUP)[None, :, :, None, None])
    o = jnp.einsum('bkgqc,bckd->bqkgd', p.astype(cv.dtype), cv)
    return o.reshape(B, C, Q_W)


def _even_mixer(h, hc, w_in, w_out, sink, ctx_out):
    B, L, _ = h.shape
    p = h @ w_in
    u_f = p[..., :FOURIER_W]
    q = p[..., FOURIER_W:EVEN_MIX_W].reshape(B, L, N_HEADS, HEAD_DIM)
    k = p[..., EVEN_MIX_W:EVEN_MIX_W + KV_W].reshape(B, L, N_KV_HEADS, HEAD_DIM)
    v = p[..., EVEN_MIX_W + KV_W:].reshape(B, L, N_KV_HEADS, HEAD_DIM)
    pc = hc @ (w_in if ctx_out else w_in[:, EVEN_MIX_W:])
    C = hc.shape[1]
    ck = pc[..., -2 * KV_W:-KV_W].reshape(B, C, N_KV_HEADS, HEAD_DIM)
    cv = pc[..., -KV_W:].reshape(B, C, N_KV_HEADS, HEAD_DIM)
    n_rows = L // GRID_W
    row = jnp.repeat(jnp.arange(n_rows, dtype=f32), GRID_W)
    col = jnp.tile(jnp.arange(GRID_W, dtype=f32), n_rows)
    q = _axial_rope(q, row, col)
    k = _axial_rope(k, row, col)
    attn = _window_attention(q, k, v, ck, cv, sink)
    out = jnp.concatenate([_fourier_mix(u_f), attn], axis=-1) @ w_out
    out_c = None
    if ctx_out:
        qc = pc[..., FOURIER_W:EVEN_MIX_W].reshape(B, C, N_HEADS, HEAD_DIM)
        out_c = jnp.concatenate([_fourier_mix(pc[..., :FOURIER_W]),
                                 _context_attention(qc, ck, cv, sink)], axis=-1) @ w_out
    return out, out_c


def _conformer_conv(h, pw1_w, pw1_b, dw_w, dw_b, ln_g, ln_b, pw2_w, pw2_b):
    a, g = jnp.split(h @ pw1_w + pw1_b, 2, axis=-1)
    u = a * jax.nn.sigmoid(g)
    u = lax.conv_general_dilated(u, dw_w[:, None, :], window_strides=(1,),
                                 padding=[(CONV_W // 2, CONV_W // 2)],
                                 dimension_numbers=('NWC', 'WIO', 'NWC'),
                                 feature_group_count=u.shape[-1]) + dw_b
    u = jax.nn.silu(_layernorm(u, ln_g, ln_b))
    return u @ pw2_w + pw2_b


def _grouped_moe(h, router_w, router_b, w_gate, w_up, w_down):
    B, L, D = h.shape
    T = B * L
    t = h.reshape(T, D)
    scores = jax.nn.sigmoid(jnp.matmul(t.astype(f32), router_w.astype(f32)))
    grouped = (scores + router_b.astype(f32)).reshape(T, N_GROUPS, EXPERTS_PER_GROUP)
    group_score = lax.top_k(grouped, GROUP_SCORE_K)[0].sum(-1)
    g_sel = jnp.argmax(group_score, axis=-1)
    in_group = jnp.take_along_axis(grouped, g_sel[:, None, None], axis=1)[:, 0]
    _, local = lax.top_k(in_group, TOP_K)
    e_idx = g_sel[:, None] * EXPERTS_PER_GROUP + local
    w = jnp.take_along_axis(scores, e_idx, axis=1)
    w = w / jnp.sum(w, axis=-1, keepdims=True)
    A = T * TOP_K
    flat_e = e_idx.reshape(A).astype(jnp.int32)
    flat_tok = jnp.repeat(jnp.arange(T, dtype=jnp.int32), TOP_K)
    flat_w = w.reshape(A).astype(h.dtype)
    order = jnp.argsort(flat_e)
    sorted_e = flat_e[order]
    counts = jnp.bincount(flat_e, length=N_EXPERTS)
    starts = jnp.cumsum(counts) - counts
    padded = (counts + MOE_BLOCK - 1) // MOE_BLOCK * MOE_BLOCK
    pends = jnp.cumsum(padded)
    pstarts = pends - padded
    dest = pstarts[sorted_e] + jnp.arange(A) - starts[sorted_e]
    n_blocks = -(-A // MOE_BLOCK) + N_EXPERTS
    n_slots = n_blocks * MOE_BLOCK
    slot_tok = jnp.full((n_slots,), T, jnp.int32).at[dest].set(flat_tok[order])
    slot_w = jnp.zeros((n_slots,), h.dtype).at[dest].set(flat_w[order])
    block_e = jnp.minimum(jnp.searchsorted(pends, jnp.arange(n_blocks) * MOE_BLOCK, side='right'),
                          N_EXPERTS - 1)
    t_pad = jnp.concatenate([t, jnp.zeros((1, D), t.dtype)], axis=0)

    def expert_block(args):
        tok, e = args
        xb = t_pad[tok]
        hid = jax.nn.silu(xb @ w_gate[e]) * (xb @ w_up[e])
        return hid @ w_down[e]

    y_slots = lax.map(expert_block, (slot_tok.reshape(n_blocks, MOE_BLOCK), block_e))
    y = jnp.zeros((T + 1, D), h.dtype).at[slot_tok].add(y_slots.reshape(n_slots, D) * slot_w[:, None])
    return y[:T].reshape(B, L, D)


def setup_inputs(seed: int = 0) -> dict:
    key = jax.random.key(seed)
    ks = jax.random.split(key, 26)
    D = D_MODEL
    nrm = lambda k, shape, s: jax.random.normal(k, shape, f32) * s
    return {
        'x': nrm(ks[0], (BATCH, SEQ, D), 1.0),
        'c': nrm(ks[1], (BATCH, D), 1.0),
        'ctx': nrm(ks[2], (BATCH, CTX_LEN, D), 1.0),
        'c_ctx': nrm(ks[3], (D,), 1.0),
        'ada_w': nrm(ks[4], (DEPTH, D, 6 * D), 0.5 * D ** -0.5),
        'ada_b': nrm(ks[5], (DEPTH, 6 * D), 0.02),
        'norm_mix_g': 1.0 + nrm(ks[6], (DEPTH, D), 0.02),
        'norm_ffn_g': 1.0 + nrm(ks[7], (DEPTH, D), 0.02),
        'even_w_in': nrm(ks[8], (N_EVEN, D, EVEN_IN_W), D ** -0.5),
        'even_w_out': nrm(ks[9], (N_EVEN, EVEN_MIX_W, D), EVEN_MIX_W ** -0.5),
        'even_sink': nrm(ks[10], (N_EVEN, N_HEADS), 0.5),
        'conv_pw1_w': nrm(ks[11], (N_ODD, D, 2 * CONV_CH), D ** -0.5),
        'conv_pw1_b': nrm(ks[12], (N_ODD, 2 * CONV_CH), 0.02),
        'conv_dw_w': nrm(ks[13], (N_ODD, CONV_W, CONV_CH), CONV_W ** -0.5),
        'conv_dw_b': nrm(ks[14], (N_ODD, CONV_CH), 0.02),
        'conv_ln_g': 1.0 + nrm(ks[15], (N_ODD, CONV_CH), 0.02),
        'conv_ln_b': nrm(ks[16], (N_ODD, CONV_CH), 0.02),
        'conv_pw2_w': nrm(ks[17], (N_ODD, CONV_CH, D), CONV_CH ** -0.5),
        'conv_pw2_b': nrm(ks[18], (N_ODD, D), 0.02),
        'router_w': nrm(ks[19], (D, N_EXPERTS), D ** -0.5),
        'router_b': nrm(ks[20], (N_EXPERTS,), 0.01),
        'moe_w_gate': nrm(ks[21], (DEPTH, N_EXPERTS, D, EXPERT_FF), D ** -0.5),
        'moe_w_up': nrm(ks[22], (DEPTH, N_EXPERTS, D, EXPERT_FF), D ** -0.5),
        'moe_w_down': nrm(ks[23], (DEPTH, N_EXPERTS, EXPERT_FF, D), EXPERT_FF ** -0.5),
        'final_norm_g': 1.0 + nrm(ks[24], (D,), 0.02),
    }


def reference(x, c, ctx, c_ctx, ada_w, ada_b, norm_mix_g, norm_ffn_g, even_w_in, even_w_out,
              even_sink, conv_pw1_w, conv_pw1_b, conv_dw_w, conv_dw_b, conv_ln_g, conv_ln_b,
              conv_pw2_w, conv_pw2_b, router_w, router_b, moe_w_gate, moe_w_up, moe_w_down,
              final_norm_g):
    last_even = DEPTH - 1 if (DEPTH - 1) % 2 == 0 else DEPTH - 2
    for i in range(DEPTH):
        j = i // 2
        is_even = i % 2 == 0
        advance_ctx = i < last_even
        sh1, sc1, g1, sh2, sc2, g2 = _adaln(c, ada_w[i], ada_b[i])
        h = _modulate(_rmsnorm(x, norm_mix_g[i]), sh1, sc1)
        if is_even or advance_ctx:
            csh1, csc1, cg1, csh2, csc2, cg2 = _adaln(c_ctx, ada_w[i], ada_b[i])
            hc = _modulate(_rmsnorm(ctx, norm_mix_g[i]), csh1, csc1)
        if is_even:
            mix, mix_c = _even_mixer(h, hc, even_w_in[j], even_w_out[j], even_sink[j], advance_ctx)
        else:
            conv_p = (conv_pw1_w[j], conv_pw1_b[j], conv_dw_w[j], conv_dw_b[j],
                      conv_ln_g[j], conv_ln_b[j], conv_pw2_w[j], conv_pw2_b[j])
            mix = _conformer_conv(h, *conv_p)
            mix_c = _conformer_conv(hc, *conv_p) if advance_ctx else None
        x = x + g1 * mix
        f = _modulate(_rmsnorm(x, norm_ffn_g[i]), sh2, sc2)
        x = x + g2 * _grouped_moe(f, router_w, router_b, moe_w_gate[i], moe_w_up[i], moe_w_down[i])
        if advance_ctx:
            ctx = ctx + cg1 * mix_c
            fc = _modulate(_rmsnorm(ctx, norm_ffn_g[i]), csh2, csc2)
            ctx = ctx + cg2 * _grouped_moe(fc, router_w, router_b, moe_w_gate[i], moe_w_up[i], moe_w_down[i])
    return _rmsnorm(x, final_norm_g)
```

```python
import contextlib
import math
import numpy as np
import concourse.bass as bass
import concourse.mybir as mybir
from concourse.bass_utils import run_bass_kernel_spmd

F32 = mybir.dt.float32
BF16 = mybir.dt.bfloat16
AF = mybir.ActivationFunctionType
ALU = mybir.AluOpType
AX = mybir.AxisListType

D = 1024
SEQ = 8192
NT = 36
NTK = 38
L0 = list(range(1, 35))
OWN = list(range(2, 34))
EPS = 1e-6
NE = 16
FF = 512


class Dep:
    __slots__ = ("w", "r")

    def __init__(self):
        self.w = {}
        self.r = {}


class Tl:
    def __init__(self, h):
        self.h = h
        self.d = Dep()


class Ring:
    def __init__(self, tiles):
        self.t = tiles
        self.i = 0

    def next(self):
        t = self.t[self.i % len(self.t)]
        self.i += 1
        return t


class Prefetch:
    def __init__(self, items, load_fn, pf):
        self.items = list(items)
        self.load = load_fn
        self.pf = pf
        self.q = {}
        self.nxt = 0

    def get(self, k):
        while self.nxt < len(self.items) and self.nxt <= k + self.pf:
            self.q[self.nxt] = self.load(self.items[self.nxt])
            self.nxt += 1
        return self.q.pop(k)


class Eng:
    def __init__(self, name, eng, sem, is_pe=False):
        self.name = name
        self.eng = eng
        self.sem = sem
        self.cnt = 0
        self.known = {}
        self.is_pe = is_pe
        self.dma_sems = []
        self.dma_tot = []
        self.rr = 0


class KB:
    def __init__(self, nc, es):
        self.nc = nc
        self.es = es
        self.uid = 0
        mk = lambda n: es.enter_context(nc.semaphore(n))
        self.PE = Eng("pe", nc.tensor, mk("s_pe"), True)
        self.ACT = Eng("act", nc.scalar, mk("s_act"))
        self.DVE = Eng("dve", nc.vector, mk("s_dve"))
        self.POOL = Eng("pool", nc.gpsimd, mk("s_pool"))
        self.SP = Eng("sp", nc.sync, mk("s_sp"))
        for Q, n in ((self.SP, 32), (self.POOL, 24)):
            Q.dma_sems = [mk(f"d_{Q.name}{i}") for i in range(n)]
            Q.dma_tot = [0] * n
        self.bar_sem = mk("s_bar")
        self.bar_cnt = 0
        self.engs = [self.PE, self.ACT, self.DVE, self.POOL, self.SP]

    def sb(self, scope, shape, dtype, name="t"):
        self.uid += 1
        return Tl(scope.enter_context(self.nc.sbuf_tensor(f"{name}_{self.uid}", list(shape), dtype)))

    def ps(self, scope, shape, dtype, name="p"):
        self.uid += 1
        return Tl(scope.enter_context(self.nc.psum_tensor(f"{name}_{self.uid}", list(shape), dtype)))

    def ring(self, scope, n, shape, dtype, name="r", psum=False):
        f = self.ps if psum else self.sb
        return Ring([f(scope, shape, dtype, name) for _ in range(n)])

    def _need(self, E, r, w):
        need = {}

        def add(dd):
            for key, sv in dd.items():
                if key not in need or need[key][1] < sv[1]:
                    need[key] = sv

        for d in r:
            add(d.w)
        for d in w:
            add(d.w)
            add(d.r)
        for key, (sem, val) in need.items():
            if E.is_pe and key == E.name:
                continue
            if E.known.get(key, 0) < val:
                E.eng.wait_ge(sem, val)
                E.known[key] = val

    @staticmethod
    def _record(key, ev, r, w):
        for d in r:
            d.r[key] = ev
        for d in w:
            d.w = {key: ev}
            d.r = {}

    def op(self, E, fn, r=(), w=()):
        self._need(E, r, w)
        ins = fn()
        E.cnt += 1
        ins.then_inc(E.sem, 1)
        self._record(E.name, (E.sem, E.cnt), r, w)

    def dma(self, Q, out_ap, in_ap, r=(), w=()):
        self._need(Q, r, w)
        k = Q.rr
        Q.rr = (k + 1) % len(Q.dma_sems)
        sem = Q.dma_sems[k]
        key = f"{Q.name}_d{k}"
        if Q.known.get(key, 0) < Q.dma_tot[k]:
            Q.eng.wait_ge(sem, Q.dma_tot[k])
            Q.known[key] = Q.dma_tot[k]
        Q.eng.dma_start(out=out_ap, in_=in_ap).then_inc(sem, 16)
        Q.dma_tot[k] += 16
        self._record(key, (sem, Q.dma_tot[k]), r, w)

    def barrier(self):
        SP = self.SP
        for E in self.engs:
            if E is SP:
                continue
            if SP.known.get(E.name, 0) < E.cnt:
                SP.eng.wait_ge(E.sem, E.cnt)
                SP.known[E.name] = E.cnt
        for Q in (self.SP, self.POOL):
            for k, sem in enumerate(Q.dma_sems):
                key = f"{Q.name}_d{k}"
                if SP.known.get(key, 0) < Q.dma_tot[k]:
                    SP.eng.wait_ge(sem, Q.dma_tot[k])
                    SP.known[key] = Q.dma_tot[k]
        self.bar_cnt += 1
        SP.eng.sem_inc(self.bar_sem, 1)
        for E in self.engs:
            if E is SP:
                continue
            E.eng.wait_ge(self.bar_sem, self.bar_cnt)
            for F in self.engs:
                E.known[F.name] = F.cnt
            for Q in (self.SP, self.POOL):
                for k in range(len(Q.dma_sems)):
                    E.known[f"{Q.name}_d{k}"] = Q.dma_tot[k]


def build_program(stop_after=None, debug=False):
    nc = bass.Bass("TRN2", target_bir_lowering=False)
    nc_v, nc_s, nc_g, nc_t = nc.vector, nc.scalar, nc.gpsimd, nc.tensor

    def din(name, shape, dt=F32):
        return nc.dram_tensor(name, list(shape), dt, kind="ExternalInput").ap()

    dbg = set(debug) if debug else set()

    def dscr(name, shape, dt):
        return nc.dram_tensor(name, list(shape), dt, kind=("ExternalOutput" if name in dbg else "Internal")).ap()

    xw = din("xw", [NT * 128, D])
    xfull = din("xfull", [SEQ, D])
    ctxb = din("ctxb", [256, D])
    cvecT = din("cvecT", [128, 8, 2])
    ada_w = din("ada_w", [2, D, 6 * D])
    ada_b = din("ada_b", [2, 6 * D])
    nmix_g = din("norm_mix_g", [2, D])
    nffn_g = din("norm_ffn_g", [2, D])
    fin_g = din("final_norm_g", [1, D])
    w_in = din("w_in", [D, 1280])
    w_in_fT = din("w_in_fT", [512, D])
    w_out = din("w_out", [D, D])
    sink = din("sink", [1, 8])
    pw1_w = din("pw1_w", [D, 2 * D])
    pw1_b = din("pw1_b", [1, 2 * D])
    dw_wT = din("dw_wT", [D, 31])
    dw_b = din("dw_b", [1, D])
    ln_g = din("ln_g", [1, D])
    ln_b = din("ln_b", [1, D])
    pw2_w = din("pw2_w", [D, D])
    pw2_b = din("pw2_b", [1, D])
    router_w = din("router_w", [D, NE])
    router_b = din("router_b", [1, NE])
    need_moe = stop_after is None or stop_after >= "E"
    moe_g = din("moe_w_gate", [2, NE, D, FF]) if need_moe else None
    moe_u = din("moe_w_up", [2, NE, D, FF]) if need_moe else None
    moe_d = din("moe_w_down", [2, NE, FF, D]) if need_moe else None
    c_ident = din("c_ident", [128, 128])
    c_fc = din("c_fc", [128, 256])
    c_f128 = din("c_f128", [3, 128, 128])
    c_tw = din("c_tw", [128, 2, 64])
    c_f64 = din("c_f64", [2, 64, NT])
    c_cos = din("c_cos", [NTK * 128, 256])
    c_sin = din("c_sin", [NTK * 128, 256])
    c_mask = din("c_mask", [2, 128, 512])
    c_tvalid = din("c_tvalid", [128, NTK])

    out = nc.dram_tensor("out", [32 * 128, D], F32, kind="ExternalOutput").ap()

    MOD = dscr("s_mod", [2, 2, 6 * D], F32)
    Wf = dscr("s_wf", [SEQ, D], BF16)
    D1 = dscr("s_d1", [128, 64, D], BF16)
    Yf = dscr("s_yf", [NT, 128, 512], BF16)
    QTs = dscr("s_qt", [NT, 64, 1024], BF16)
    X1 = dscr("s_x1", [NT * 128, D], F32)
    FTs = dscr("s_ft", [NT, 128, 1024], BF16)
    X2 = dscr("s_x2", [NT * 128, D], F32)
    UTs = dscr("s_ut", [128, 8, 34 * 128], BF16)
    X3 = dscr("s_x3", [NT * 128, D], F32)

    phases_done = []

    with contextlib.ExitStack() as es:
        kb = KB(nc, es)
        PE, ACT, DVE, POOL, SP = kb.PE, kb.ACT, kb.DVE, kb.POOL, kb.SP

        identb = kb.sb(es, [128, 128], BF16, "identb")
        identf = kb.sb(es, [128, 128], F32, "identf")
        epst = kb.sb(es, [128, 1], F32, "eps")
        tvalid = kb.sb(es, [128, NTK], F32, "tvalid")
        ones2 = kb.sb(es, [128, 2, 1], F32, "ones2")
        wts = [kb.sb(es, [128, NT, NE], F32, f"wts{i}") for i in range(2)]
        kb.dma(POOL, identb.h[:], c_ident, w=[identb.d])
        kb.dma(SP, identf.h[:], c_ident, w=[identf.d])
        kb.dma(SP, tvalid.h[:], c_tvalid, w=[tvalid.d])
        kb.op(POOL, lambda: nc_g.memset(epst.h[:], EPS), w=[epst.d])
        kb.op(POOL, lambda: nc_g.memset(ones2.h[:], 1.0), w=[ones2.d])

        def done(name):
            phases_done.append(name)
            kb.barrier()
            return stop_after == name

        def bc_load(dst, row_ap):
            kb.dma(SP, dst.h[:], row_ap.partition_broadcast(128), w=[dst.d])

        def prep_mod(scope, layer, row, which, ncx):
            res = {}
            m = MOD[layer, row:row + 1, :]
            for nm in which:
                t = kb.sb(scope, [128, D], F32, "mod" + nm)
                if nm in ("G1", "G2"):
                    tmp = ncx.tmp.next()
                    gt = ncx.tmp.next()
                    off = 1 * D if nm == "G1" else 4 * D
                    gsrc = nmix_g if nm == "G1" else nffn_g
                    bc_load(tmp, m[:, off:off + D])
                    bc_load(gt, gsrc[layer:layer + 1, :])
                    kb.op(DVE, lambda t=t, tmp=tmp, gt=gt: nc_v.scalar_tensor_tensor(
                        out=t.h[:], in0=tmp.h[:], scalar=1.0, in1=gt.h[:], op0=ALU.add, op1=ALU.mult),
                          r=[tmp.d, gt.d], w=[t.d])
                else:
                    off = {"S1": 0, "g1": 2 * D, "S2": 3 * D, "g2": 5 * D}[nm]
                    bc_load(t, m[:, off:off + D])
                res[nm] = t
            return res

        class NormCtx:
            def __init__(self, scope, depth=3):
                self.junk = kb.ring(scope, depth, [128, D], BF16, "junk")
                self.stat = kb.ring(scope, 2 * depth, [128, 4], F32, "nstat")
                self.tmp = kb.ring(scope, depth, [128, D], F32, "ntmp")

        def rstd_of(ncx, x_ap, xdeps):
            junk = ncx.junk.next()
            st = ncx.stat.next()
            kb.op(ACT, lambda: nc_s.activation(out=junk.h[:], in_=x_ap, func=AF.Square, scale=1.0 / 32.0,
                                               accum_out=st.h[:, 0:1]), r=xdeps, w=[junk.d, st.d])
            kb.op(ACT, lambda: nc_s.activation(out=st.h[:, 1:2], in_=st.h[:, 0:1], func=AF.Sqrt,
                                               bias=epst.h[:, 0:1], scale=1.0), r=[epst.d], w=[st.d])
            kb.op(DVE, lambda: nc_v.reciprocal(out=st.h[:, 2:3], in_=st.h[:, 1:2]), w=[st.d])
            return st

        def norm_mod(ncx, x_ap, xdeps, G, S, out_ap, outdeps):
            st = rstd_of(ncx, x_ap, xdeps)
            tmp = ncx.tmp.next()
            kb.op(DVE, lambda: nc_v.scalar_tensor_tensor(out=tmp.h[:], in0=x_ap, scalar=st.h[:, 2:3], in1=G.h[:],
                                                         op0=ALU.mult, op1=ALU.mult),
                  r=list(xdeps) + [st.d, G.d], w=[tmp.d])
            kb.op(POOL, lambda: nc_g.tensor_tensor(out=out_ap, in0=tmp.h[:], in1=S.h[:], op=ALU.add),
                  r=[tmp.d, S.d], w=outdeps)

        def transposes_bf(psb_ring, srcs, rdeps, dst_ap, wdeps, evac, rows=128):
            pst = psb_ring.next()
            n = len(srcs)

            def f():
                ins = None
                for k, ap in enumerate(srcs):
                    ins = nc_t.transpose(out=pst.h[0:rows, k * 128:(k + 1) * 128], in_=ap, identity=identb.h[:])
                return ins

            kb.op(PE, f, r=list(rdeps) + [identb.d], w=[pst.d])
            if evac is ACT:
                kb.op(ACT, lambda: nc_s.copy(out=dst_ap, in_=pst.h[0:rows, 0:n * 128]), r=[pst.d], w=wdeps)
            else:
                kb.op(DVE, lambda: nc_v.tensor_copy(out=dst_ap, in_=pst.h[0:rows, 0:n * 128]), r=[pst.d], w=wdeps)

        def linear(ps_ap, psd, hT, W, col0, ncols, extra_r=()):
            def f():
                ins = None
                for k in range(8):
                    ins = nc_t.matmul(ps_ap, lhsT=hT.h[:, k, :], rhs=W.h[:, k, col0:col0 + ncols],
                                      start=(k == 0), stop=(k == 7))
                return ins

            kb.op(PE, f, r=[hT.d, W.d] + list(extra_r), w=[psd])

        def load_w_bf(dst, src_ap):
            kb.dma(POOL, dst.h[:], src_ap.rearrange("(k p) n -> p k n", p=128), w=[dst.d])

        with contextlib.ExitStack() as ph:
            scT = kb.sb(ph, [128, 8, 2], F32, "scT")
            kb.dma(SP, scT.h[:], cvecT, w=[scT.d])
            kb.op(ACT, lambda: nc_s.activation(out=scT.h[:], in_=scT.h[:], func=AF.Silu), w=[scT.d])
            wring = kb.ring(ph, 2, [128, 8, 512], F32, "adaw")
            psr = kb.ring(ph, 2, [128, 512], F32, "psA", psum=True)
            for li in range(2):
                bias = kb.sb(ph, [2, 6 * D], F32, "adab")
                msb = kb.sb(ph, [2, 6 * D], F32, "adam")
                kb.dma(SP, bias.h[:], ada_b[li:li + 1, :].partition_broadcast(2), w=[bias.d])
                for j in range(12):
                    wt = wring.next()
                    kb.dma(SP, wt.h[:], ada_w[li, :, j * 512:(j + 1) * 512].rearrange("(k p) n -> p k n", p=128),
                           w=[wt.d])
                    ps = psr.next()

                    def f(wt=wt, ps=ps):
                        ins = None
                        for k in range(8):
                            ins = nc_t.matmul(ps.h[0:2, :], lhsT=scT.h[:, k, :], rhs=wt.h[:, k, :],
                                              start=(k == 0), stop=(k == 7))
                        return ins

                    kb.op(PE, f, r=[scT.d, wt.d], w=[ps.d])
                    kb.op(DVE, lambda ps=ps, j=j: nc_v.tensor_tensor(out=msb.h[:, j * 512:(j + 1) * 512],
                                                                     in0=ps.h[0:2, :],
                                                                     in1=bias.h[:, j * 512:(j + 1) * 512], op=ALU.add),
                          r=[ps.d, bias.d], w=[msb.d])
                kb.dma(SP, MOD[li], msb.h[:], r=[msb.d])
        if done("A"):
            return nc, phases_done

        with contextlib.ExitStack() as lay:
            with contextlib.ExitStack() as ph:
                Wp = kb.sb(ph, [128, 8, D], BF16, "Wp")
                psf = kb.ring(ph, 6, [128, 512], F32, "psB", psum=True)
                psb = kb.ring(ph, 2, [128, 1024], BF16, "psBb", psum=True)
                with contextlib.ExitStack() as sub:
                    wfT = kb.sb(sub, [128, 4, D], BF16, "wfT")
                    fct = kb.sb(sub, [128, 256], BF16, "fct")
                    kb.dma(POOL, wfT.h[:], w_in_fT.rearrange("(g c) d -> c g d", c=128), w=[wfT.d])
                    kb.dma(POOL, fct.h[:], c_fc, w=[fct.d])
                    for dk in range(8):
                        for gp in range(2):
                            ps = psf.next()

                            def f(ps=ps, dk=dk, gp=gp):
                                ins = None
                                for gg in range(2):
                                    g = gp * 2 + gg
                                    ins = nc_t.matmul(ps.h[:, gg * 256:(gg + 1) * 256],
                                                      lhsT=wfT.h[:, g, dk * 128:(dk + 1) * 128], rhs=fct.h[:],
                                                      start=True, stop=True)
                                return ins

                            kb.op(PE, f, r=[wfT.d, fct.d], w=[ps.d])
                            kb.op(DVE, lambda ps=ps, dk=dk, gp=gp: nc_v.tensor_copy(
                                out=Wp.h[:, dk, gp * 512:(gp + 1) * 512], in_=ps.h[:]), r=[ps.d], w=[Wp.d])
                    kb.barrier()
                    if stop_after == "B0":
                        return nc, phases_done
                with contextlib.ExitStack() as sub:
                    ncx = NormCtx(sub)
                    mod = prep_mod(sub, 0, 0, ["G1", "S1"], ncx)
                    xr = kb.ring(sub, 4, [128, D], F32, "xB")
                    hr = kb.ring(sub, 3, [128, D], BF16, "hB")
                    hTr = kb.ring(sub, 3, [128, 8, 128], BF16, "hTB")
                    wr = kb.ring(sub, 3, [128, D], BF16, "wB")
                    def ld_b1(t):
                        xt = xr.next()
                        kb.dma(SP, xt.h[:], xfull[t * 128:(t + 1) * 128, :], w=[xt.d])
                        return xt

                    pfx = Prefetch(range(64), ld_b1, 2)
                    for t in range(64):
                        xt = pfx.get(t)
                        h = hr.next()
                        norm_mod(ncx, xt.h[:], [xt.d], mod["G1"], mod["S1"], h.h[:], [h.d])
                        hT = hTr.next()
                        transposes_bf(psb, [h.h[:, k * 128:(k + 1) * 128] for k in range(8)], [h.d],
                                      hT.h[:].rearrange("p k t -> p (k t)"), [hT.d], ACT)
                        wt = wr.next()
                        for c in range(2):
                            ps = psf.next()
                            linear(ps.h[:], ps.d, hT, Wp, c * 512, 512)
                            if c == 0:
                                kb.op(ACT, lambda ps=ps, wt=wt: nc_s.copy(out=wt.h[:, 0:512], in_=ps.h[:]),
                                      r=[ps.d], w=[wt.d])
                            else:
                                kb.op(DVE, lambda ps=ps, wt=wt: nc_v.tensor_copy(out=wt.h[:, 512:1024], in_=ps.h[:]),
                                      r=[ps.d], w=[wt.d])
                        kb.dma(SP, Wf[t * 128:(t + 1) * 128, :], wt.h[:], r=[wt.d])
                    kb.barrier()
                    if stop_after == "B1":
                        return nc, phases_done
                with contextlib.ExitStack() as sub:
                    f128 = kb.sb(sub, [128, 3, 128], BF16, "f128")
                    tw = kb.sb(sub, [128, 2, 64], F32, "tw")
                    kb.dma(POOL, f128.h[:], c_f128.rearrange("a p n -> p a n"), w=[f128.d])
                    kb.dma(SP, tw.h[:], c_tw, w=[tw.d])
                    d0r = kb.ring(sub, 4, [128, D], BF16, "d0")
                    d1r = kb.ring(sub, 3, [128, D], BF16, "d1")
                    t1r = kb.ring(sub, 3, [128, 512], F32, "t1")
                    t2r = kb.ring(sub, 3, [128, 512], F32, "t2")
                    Wf_v = Wf.rearrange("(l1 l2) c -> l2 l1 c", l2=64)
                    def ld_b2(l2):
                        d0 = d0r.next()
                        kb.dma(SP, d0.h[:], Wf_v[l2], w=[d0.d])
                        return d0

                    pfx = Prefetch(range(64), ld_b2, 2)
                    for l2 in range(64):
                        d0 = pfx.get(l2)
                        dv = d0.h[:].rearrange("p (g r c) -> p g r c", g=4, r=2)
                        Dr = dv[:, :, 0, :]
                        Di = dv[:, :, 1, :]
                        psr_ = psf.next()
                        psi_ = psf.next()

                        def f(psr_=psr_, psi_=psi_, Dr=Dr, Di=Di):
                            nc_t.matmul(psr_.h[:], lhsT=f128.h[:, 0, :], rhs=Dr, start=True, stop=False)
                            nc_t.matmul(psr_.h[:], lhsT=f128.h[:, 1, :], rhs=Di, start=False, stop=True)
                            nc_t.matmul(psi_.h[:], lhsT=f128.h[:, 0, :], rhs=Di, start=True, stop=False)
                            return nc_t.matmul(psi_.h[:], lhsT=f128.h[:, 2, :], rhs=Dr, start=False, stop=True)

                        kb.op(PE, f, r=[d0.d, f128.d], w=[psr_.d, psi_.d])
                        t1 = t1r.next()
                        t2 = t2r.next()
                        d1 = d1r.next()
                        kb.op(ACT, lambda t1=t1, psi_=psi_, l2=l2: nc_s.activation(
                            out=t1.h[:], in_=psi_.h[:], func=AF.Identity, scale=tw.h[:, 1, l2:l2 + 1]),
                              r=[psi_.d, tw.d], w=[t1.d])
                        kb.op(ACT, lambda t2=t2, psi_=psi_, l2=l2: nc_s.activation(
                            out=t2.h[:], in_=psi_.h[:], func=AF.Identity, scale=tw.h[:, 0, l2:l2 + 1]),
                              r=[psi_.d, tw.d], w=[t2.d])
                        kb.op(DVE, lambda d1=d1, psr_=psr_, t1=t1, l2=l2: nc_v.scalar_tensor_tensor(
                            out=d1.h[:, 0:512], in0=psr_.h[:], scalar=tw.h[:, 0, l2:l2 + 1], in1=t1.h[:],
                            op0=ALU.mult, op1=ALU.subtract), r=[psr_.d, t1.d, tw.d], w=[d1.d])
                        kb.op(DVE, lambda d1=d1, psr_=psr_, t2=t2, l2=l2: nc_v.scalar_tensor_tensor(
                            out=d1.h[:, 512:1024], in0=psr_.h[:], scalar=tw.h[:, 1, l2:l2 + 1], in1=t2.h[:],
                            op0=ALU.mult, op1=ALU.add), r=[psr_.d, t2.d, tw.d], w=[d1.d])
                        kb.dma(SP, D1[:, l2, :], d1.h[:], r=[d1.d])
                    kb.barrier()
                    if stop_after == "B2":
                        return nc, phases_done
                with contextlib.ExitStack() as sub:
                    f64 = kb.sb(sub, [64, 2, NT], BF16, "f64")
                    kb.dma(POOL, f64.h[:], c_f64.rearrange("a p n -> p a n"), w=[f64.d])
                    dr = kb.ring(sub, 3, [64, 4, D], BF16, "d3")
                    yr = kb.ring(sub, 3, [NT, 4, 512], BF16, "y3")
                    def ld_b3(kbk):
                        dd = dr.next()
                        kb.dma(SP, dd.h[:], D1[kbk * 4:(kbk + 1) * 4, :, :].rearrange("k l c -> l k c"), w=[dd.d])
                        return dd

                    pfx = Prefetch(range(32), ld_b3, 2)
                    for kbk in range(32):
                        dd = pfx.get(kbk)
                        yt = yr.next()
                        for kk in range(4):
                            ps = psf.next()

                            def f(ps=ps, dd=dd, kk=kk):
                                nc_t.matmul(ps.h[0:NT, :], lhsT=f64.h[:, 0, :], rhs=dd.h[:, kk, 0:512],
                                            start=True, stop=False)
                                return nc_t.matmul(ps.h[0:NT, :], lhsT=f64.h[:, 1, :], rhs=dd.h[:, kk, 512:1024],
                                                   start=False, stop=True)

                            kb.op(PE, f, r=[dd.d, f64.d], w=[ps.d])
                            if kk % 2 == 0:
                                kb.op(ACT, lambda ps=ps, yt=yt, kk=kk: nc_s.copy(out=yt.h[:, kk, :], in_=ps.h[0:NT, :]),
                                      r=[ps.d], w=[yt.d])
                            else:
                                kb.op(DVE, lambda ps=ps, yt=yt, kk=kk: nc_v.tensor_copy(out=yt.h[:, kk, :],
                                                                                        in_=ps.h[0:NT, :]),
                                      r=[ps.d], w=[yt.d])
                        kb.dma(SP, Yf[:, kbk * 4:(kbk + 1) * 4, :], yt.h[:], r=[yt.d])
            if done("B"):
                return nc, phases_done

            with contextlib.ExitStack() as cd:
                KT = kb.sb(cd, [64, NTK, 2, 128], BF16, "KT")
                Vaug = kb.sb(cd, [128, NTK, 2, 65], BF16, "Vaug")
                KTd = [Dep() for _ in range(NTK)]
                Vd = [Dep() for _ in range(NTK)]
                with contextlib.ExitStack() as ph:
                    ncx = NormCtx(ph)
                    mod = prep_mod(ph, 0, 0, ["G1", "S1"], ncx)
                    modc = prep_mod(ph, 0, 1, ["G1", "S1"], ncx)
                    Wqkv = kb.sb(ph, [128, 8, 768], BF16, "Wqkv")
                    load_w_bf(Wqkv, w_in[:, 512:1280])
                    psf = kb.ring(ph, 5, [128, 512], F32, "psC", psum=True)
                    psb = kb.ring(ph, 3, [128, 1024], BF16, "psCb", psum=True)
                    xr = kb.ring(ph, 3, [128, D], F32, "xC")
                    hr = kb.ring(ph, 3, [128, D], BF16, "hC")
                    hTr = kb.ring(ph, 3, [128, 8, 128], BF16, "hTC")
                    cosr = kb.ring(ph, 3, [128, 256], F32, "cosC")
                    sinr = kb.ring(ph, 3, [128, 256], F32, "sinC")
                    ta = kb.ring(ph, 4, [128, 256], F32, "ropA")
                    tb = kb.ring(ph, 4, [128, 256], F32, "ropB")
                    qrr = kb.ring(ph, 3, [128, 512], BF16, "qr")
                    krr = kb.ring(ph, 3, [128, 128], BF16, "kr")
                    qtr = kb.ring(ph, 3, [64, 1024], BF16, "qtC")
                    def ld_c(i):
                        xt = xr.next()
                        src = xw[i * 128:(i + 1) * 128, :] if i < NT else ctxb[(i - NT) * 128:(i - NT + 1) * 128, :]
                        kb.dma(SP, xt.h[:], src, w=[xt.d])
                        cs = cosr.next()
                        sn = sinr.next()
                        kb.dma(SP, cs.h[:], c_cos[i * 128:(i + 1) * 128, :], w=[cs.d])
                        kb.dma(SP, sn.h[:], c_sin[i * 128:(i + 1) * 128, :], w=[sn.d])
                        return xt, cs, sn

                    pfx = Prefetch(range(NTK), ld_c, 2)
                    for i in range(NTK):
                        xt, cs, sn = pfx.get(i)
                        m = mod if i < NT else modc
                        h = hr.next()
                        norm_mod(ncx, xt.h[:], [xt.d], m["G1"], m["S1"], h.h[:], [h.d])
                        hT = hTr.next()
                        transposes_bf(psb, [h.h[:, k * 128:(k + 1) * 128] for k in range(8)], [h.d],
                                      hT.h[:].rearrange("p k t -> p (k t)"), [hT.d], ACT)
                        psq = psf.next()
                        pskv = psf.next()
                        linear(psq.h[:], psq.d, hT, Wqkv, 0, 512)
                        linear(pskv.h[:, 0:256], pskv.d, hT, Wqkv, 512, 256)
                        qr = qrr.next()
                        kr = krr.next()
                        for (src_ap, srcd, dstt, na) in ((psq.h[:, 0:512], psq.d, qr, 16), (pskv.h[:, 0:128], pskv.d, kr, 4)):
                            sv = src_ap.rearrange("p (a two f) -> p a two f", two=2, f=16)
                            x1 = sv[:, :, 0, :]
                            x2 = sv[:, :, 1, :]
                            dv = dstt.h[:].rearrange("p (a two f) -> p a two f", two=2, f=16)
                            cv = cs.h[:, 0:na * 16].rearrange("p (a f) -> p a f", f=16)
                            svn = sn.h[:, 0:na * 16].rearrange("p (a f) -> p a f", f=16)
                            A = ta.next()
                            B = tb.next()
                            Av = A.h[:, 0:na * 16].rearrange("p (a f) -> p a f", f=16)
                            Bv = B.h[:, 0:na * 16].rearrange("p (a f) -> p a f", f=16)
                            kb.op(DVE, lambda Av=Av, x1=x1, cv=cv: nc_v.tensor_tensor(out=Av, in0=x1, in1=cv, op=ALU.mult),
                                  r=[srcd, cs.d], w=[A.d])
                            kb.op(DVE, lambda Bv=Bv, x2=x2, svn=svn: nc_v.tensor_tensor(out=Bv, in0=x2, in1=svn, op=ALU.mult),
                                  r=[srcd, sn.d], w=[B.d])
                            kb.op(POOL, lambda dv=dv, Av=Av, Bv=Bv: nc_g.tensor_tensor(out=dv[:, :, 0, :], in0=Av, in1=Bv,
                                                                                       op=ALU.subtract),
                                  r=[A.d, B.d], w=[dstt.d])
                            A2 = ta.next()
                            B2 = tb.next()
                            A2v = A2.h[:, 0:na * 16].rearrange("p (a f) -> p a f", f=16)
                            B2v = B2.h[:, 0:na * 16].rearrange("p (a f) -> p a f", f=16)
                            kb.op(DVE, lambda A2v=A2v, x1=x1, svn=svn: nc_v.tensor_tensor(out=A2v, in0=x1, in1=svn, op=ALU.mult),
                                  r=[srcd, sn.d], w=[A2.d])
                            kb.op(DVE, lambda B2v=B2v, x2=x2, cv=cv: nc_v.tensor_tensor(out=B2v, in0=x2, in1=cv, op=ALU.mult),
                                  r=[srcd, cs.d], w=[B2.d])
                            kb.op(POOL, lambda dv=dv, A2v=A2v, B2v=B2v: nc_g.tensor_tensor(out=dv[:, :, 1, :], in0=A2v, in1=B2v,
                                                                                          op=ALU.add),
                                  r=[A2.d, B2.d], w=[dstt.d])
                        kb.op(DVE, lambda i=i, pskv=pskv: nc_v.tensor_scalar(
                            out=Vaug.h[:, i, :, 0:64], in0=pskv.h[:, 128:256].rearrange("p (a d) -> p a d", a=2),
                            scalar1=tvalid.h[:, i:i + 1], scalar2=None, op0=ALU.mult),
                              r=[pskv.d, tvalid.d], w=[Vd[i]])
                        kb.op(POOL, lambda i=i: nc_g.tensor_scalar(
                            out=Vaug.h[:, i, :, 64:65], in0=ones2.h[:], scalar1=tvalid.h[:, i:i + 1], scalar2=None,
                            op0=ALU.mult), r=[ones2.d, tvalid.d], w=[Vd[i]])
                        qt = qtr.next()
                        transposes_bf(psb, [qr.h[:, hh * 64:(hh + 1) * 64] for hh in range(8)], [qr.d],
                                      qt.h[:], [qt.d], ACT, rows=64)
                        transposes_bf(psb, [kr.h[:, hh * 64:(hh + 1) * 64] for hh in range(2)], [kr.d],
                                      KT.h[:, i, :, :].rearrange("p a t -> p (a t)"), [KTd[i]], DVE, rows=64)
                        if i < NT:
                            kb.dma(SP, QTs[i], qt.h[:], r=[qt.d])
                if done("C"):
                    return nc, phases_done

                with contextlib.ExitStack() as ph:
                    post = PostMix(kb, nc, ph, identf, epst, depth=3)
                    Wout = kb.sb(ph, [128, 8, D], BF16, "Wout")
                    load_w_bf(Wout, w_out)
                    masks = kb.sb(ph, [128, 2, 512], BF16, "masks")
                    kb.dma(POOL, masks.h[:], c_mask.rearrange("a p n -> p a n"), w=[masks.d])
                    esink = kb.sb(ph, [128, 8], F32, "esink")
                    kb.dma(SP, esink.h[:], sink.partition_broadcast(128), w=[esink.d])
                    kb.op(ACT, lambda: nc_s.activation(out=esink.h[:], in_=esink.h[:], func=AF.Exp), w=[esink.d])
                    psf = kb.ring(ph, 6, [128, 512], F32, "psD", psum=True)
                    psb = kb.ring(ph, 2, [128, 1024], BF16, "psDb", psum=True)
                    post.setup(router_w, router_b, psf)
                    ncx = NormCtx(ph)
                    mod = prep_mod(ph, 0, 0, ["g1", "G2", "S2"], ncx)
                    xr = kb.ring(ph, 3, [128, D], F32, "xD")
                    qtr = kb.ring(ph, 3, [64, 1024], BF16, "qtD")
                    mcr = kb.ring(ph, 3, [128, D], BF16, "mcD")
                    mcTr = kb.ring(ph, 3, [128, 8, 128], BF16, "mcT")
                    ptr = kb.ring(ph, 15, [128, 512], BF16, "pt")
                    denr = kb.ring(ph, 4, [128, 8], F32, "den")
                    zf = xr.next()
                    zb = mcr.next()
                    kb.op(POOL, lambda: nc_g.memset(zf.h[:], 0.0), w=[zf.d])
                    kb.op(POOL, lambda: nc_g.memset(zb.h[:], 0.0), w=[zb.d])
                    for zi in (0, NT - 1):
                        kb.dma(SP, X1[zi * 128:(zi + 1) * 128, :], zf.h[:], r=[zf.d])
                        kb.dma(SP, FTs[zi], zb.h[:], r=[zb.d])
                    def ld_d(i):
                        xt = xr.next()
                        kb.dma(SP, xt.h[:], xw[i * 128:(i + 1) * 128, :], w=[xt.d])
                        qt = qtr.next()
                        kb.dma(SP, qt.h[:], QTs[i], w=[qt.d])
                        mc = mcr.next()
                        kb.dma(SP, mc.h[:, 0:512], Yf[i], w=[mc.d])
                        return xt, qt, mc

                    pfx = Prefetch(L0, ld_d, 2)
                    for ki, i in enumerate(L0):
                        xt, qt, mc = pfx.get(ki)
                        for kv in range(2):
                            keys = [36, 37, i - 1, i, i + 1]
                            pts = []
                            for jj, j in enumerate(keys):
                                pss = psf.next()
                                kb.op(PE, lambda pss=pss, j=j, kv=kv, qt=qt: nc_t.matmul(
                                    pss.h[:], lhsT=KT.h[:, j, kv, :], rhs=qt.h[:, kv * 512:(kv + 1) * 512],
                                    start=True, stop=True), r=[KTd[j], qt.d], w=[pss.d])
                                pt = ptr.next()
                                kb.op(ACT, lambda pss=pss, pt=pt: nc_s.activation(out=pt.h[:], in_=pss.h[:], func=AF.Exp,
                                                                                   scale=0.125), r=[pss.d], w=[pt.d])
                                if jj in (2, 4):
                                    mi = 0 if jj == 2 else 1
                                    kb.op(POOL, lambda pt=pt, mi=mi: nc_g.tensor_tensor(out=pt.h[:], in0=pt.h[:],
                                                                                        in1=masks.h[:, mi, :], op=ALU.mult),
                                          r=[masks.d], w=[pt.d])
                                pts.append(pt)
                            pso = psf.next()

                            def f(pso=pso, pts=pts, keys=keys, kv=kv):
                                ins = None
                                for g in range(4):
                                    for jj, j in enumerate(keys):
                                        ins = nc_t.matmul(pso.h[:, g * 65:(g + 1) * 65],
                                                          lhsT=pts[jj].h[:, g * 128:(g + 1) * 128],
                                                          rhs=Vaug.h[:, j, kv, :], start=(jj == 0), stop=(jj == 4))
                                return ins

                            kb.op(PE, f, r=[p.d for p in pts] + [Vd[j] for j in keys], w=[pso.d])
                            den = denr.next()
                            pov = pso.h[:, 0:260].rearrange("p (g e) -> p g e", e=65)
                            kb.op(DVE, lambda den=den, pov=pov, kv=kv: nc_v.tensor_tensor(
                                out=den.h[:, 0:4], in0=pov[:, :, 64], in1=esink.h[:, kv * 4:(kv + 1) * 4], op=ALU.add),
                                  r=[pso.d, esink.d], w=[den.d])
                            kb.op(DVE, lambda den=den: nc_v.reciprocal(out=den.h[:, 4:8], in_=den.h[:, 0:4]), w=[den.d])
                            for g in range(4):
                                hh = kv * 4 + g
                                kb.op(DVE, lambda mc=mc, pso=pso, den=den, g=g, hh=hh: nc_v.tensor_scalar(
                                    out=mc.h[:, 512 + hh * 64:512 + (hh + 1) * 64], in0=pso.h[:, g * 65:g * 65 + 64],
                                    scalar1=den.h[:, 4 + g:5 + g], scalar2=None, op0=ALU.mult),
                                      r=[pso.d, den.d], w=[mc.d])
                        mcT = mcTr.next()
                        transposes_bf(psb, [mc.h[:, k * 128:(k + 1) * 128] for k in range(8)], [mc.d],
                                      mcT.h[:].rearrange("p k t -> p (k t)"), [mcT.d], ACT)
                        pm = [psf.next(), psf.next()]
                        for c in range(2):
                            linear(pm[c].h[:], pm[c].d, mcT, Wout, c * 512, 512)
                        post.run(i, pm, None, xt, mod, ncx, X1, FTs, wts[0])
                    post.routing(wts[0])
                if done("D"):
                    return nc, phases_done

            moe_phase(kb, nc, 0, list(range(NT)), MOD[0, 0:1, 5 * D:6 * D], X1, X2, FTs, wts[0], moe_g, moe_u, moe_d, None, None, epst)
            if done("E"):
                return nc, phases_done

        with contextlib.ExitStack() as lay:
            convsc = contextlib.ExitStack()
            DG = kb.sb(convsc, [128, 8, 31, 128], BF16, "DG")
            dwT = kb.sb(convsc, [128, 8, 31], F32, "dwT")
            kb.dma(SP, dwT.h[:], dw_wT.rearrange("(c p) j -> p c j", p=128), w=[dwT.d])
            DGd = [Dep() for _ in range(8 * 31)]
            dg_todo = [(c, j) for c in range(8) for j in range(31)]

            def emit_dg(n):
                for _ in range(n):
                    if not dg_todo:
                        return
                    c, j = dg_todo.pop(0)
                    kb.op(ACT, lambda c=c, j=j: nc_s.activation(out=DG.h[:, c, j, :], in_=identb.h[:], func=AF.Identity,
                                                                scale=dwT.h[:, c, j:j + 1]),
                          r=[identb.d, dwT.d], w=[DGd[c * 31 + j]])
            with contextlib.ExitStack() as ph:
                Wpw1 = kb.sb(ph, [128, 8, 2 * D], BF16, "Wpw1")
                load_w_bf(Wpw1, pw1_w)
                b1 = kb.sb(ph, [128, 2 * D], F32, "b1")
                bc_load(b1, pw1_b)
                psf = kb.ring(ph, 6, [128, 512], F32, "psF", psum=True)
                psb = kb.ring(ph, 2, [128, 1024], BF16, "psFb", psum=True)
                ncx = NormCtx(ph)
                mod = prep_mod(ph, 1, 0, ["G1", "S1"], ncx)
                xr = kb.ring(ph, 3, [128, D], F32, "xF")
                hr = kb.ring(ph, 3, [128, D], BF16, "hF")
                hTr = kb.ring(ph, 3, [128, 8, 128], BF16, "hTF")
                tgr = kb.ring(ph, 3, [128, 512], F32, "tg")
                sgr = kb.ring(ph, 3, [128, 512], F32, "sg")
                tar = kb.ring(ph, 3, [128, 512], F32, "taF")
                tur = kb.ring(ph, 3, [128, 512], F32, "tuF")
                ur = kb.ring(ph, 3, [128, D], BF16, "uF")
                uTr = kb.ring(ph, 3, [128, 8, 128], BF16, "uTF")
                def ld_f(i):
                    xt = xr.next()
                    kb.dma(SP, xt.h[:], X2[i * 128:(i + 1) * 128, :], w=[xt.d])
                    return xt

                pfx = Prefetch(L0, ld_f, 2)
                for ki, i in enumerate(L0):
                    xt = pfx.get(ki)
                    emit_dg(8)
                    h = hr.next()
                    norm_mod(ncx, xt.h[:], [xt.d], mod["G1"], mod["S1"], h.h[:], [h.d])
                    hT = hTr.next()
                    transposes_bf(psb, [h.h[:, k * 128:(k + 1) * 128] for k in range(8)], [h.d],
                                  hT.h[:].rearrange("p k t -> p (k t)"), [hT.d], ACT)
                    pp = [psf.next() for _ in range(4)]
                    for c in range(4):
                        linear(pp[c].h[:], pp[c].d, hT, Wpw1, c * 512, 512)
                    u = ur.next()
                    for hf in range(2):
                        tg = tgr.next()
                        sg = sgr.next()
                        tA = tar.next()
                        tu = tur.next()
                        kb.op(DVE, lambda tg=tg, hf=hf, pp=pp: nc_v.tensor_tensor(
                            out=tg.h[:], in0=pp[2 + hf].h[:], in1=b1.h[:, D + hf * 512:D + (hf + 1) * 512], op=ALU.add),
                              r=[pp[2 + hf].d, b1.d], w=[tg.d])
                        kb.op(ACT, lambda tg=tg, sg=sg: nc_s.activation(out=sg.h[:], in_=tg.h[:], func=AF.Sigmoid),
                              r=[tg.d], w=[sg.d])
                        kb.op(DVE, lambda tA=tA, hf=hf, pp=pp: nc_v.tensor_tensor(
                            out=tA.h[:], in0=pp[hf].h[:], in1=b1.h[:, hf * 512:(hf + 1) * 512], op=ALU.add),
                              r=[pp[hf].d, b1.d], w=[tA.d])
                        if i in OWN:
                            kb.op(POOL, lambda u=u, tA=tA, sg=sg, hf=hf: nc_g.tensor_tensor(
                                out=u.h[:, hf * 512:(hf + 1) * 512], in0=tA.h[:], in1=sg.h[:], op=ALU.mult),
                                  r=[tA.d, sg.d], w=[u.d])
                        else:
                            kb.op(POOL, lambda tu=tu, tA=tA, sg=sg: nc_g.tensor_tensor(out=tu.h[:], in0=tA.h[:], in1=sg.h[:],
                                                                                       op=ALU.mult), r=[tA.d, sg.d], w=[tu.d])
                            kb.op(POOL, lambda u=u, tu=tu, hf=hf, i=i: nc_g.tensor_scalar(
                                out=u.h[:, hf * 512:(hf + 1) * 512], in0=tu.h[:], scalar1=tvalid.h[:, i:i + 1], scalar2=None,
                                op0=ALU.mult), r=[tu.d, tvalid.d], w=[u.d])
                    uT = uTr.next()
                    transposes_bf(psb, [u.h[:, k * 128:(k + 1) * 128] for k in range(8)], [u.d],
                                  uT.h[:].rearrange("p k t -> p (k t)"), [uT.d], ACT)
                    kb.dma(SP, UTs[:, :, (i - 1) * 128:i * 128], uT.h[:], r=[uT.d])
                emit_dg(8 * 31)
            if done("F"):
                return nc, phases_done

            with contextlib.ExitStack() as ph:
                post = PostMix(kb, nc, ph, identf, epst)
                Wpw2 = kb.sb(ph, [128, 8, D], BF16, "Wpw2")
                load_w_bf(Wpw2, pw2_w)
                dwb = kb.sb(ph, [128, D], F32, "dwb")
                lng = kb.sb(ph, [128, D], F32, "lng")
                lnb = kb.sb(ph, [128, D], F32, "lnb")
                b2 = kb.sb(ph, [128, D], F32, "b2")
                bc_load(dwb, dw_b)
                bc_load(lng, ln_g)
                bc_load(lnb, ln_b)
                bc_load(b2, pw2_b)
                psf = kb.ring(ph, 6, [128, 512], F32, "psG", psum=True)
                psb = kb.ring(ph, 2, [128, 1024], BF16, "psGb", psum=True)
                post.setup(router_w, router_b, psf)
                ncx = NormCtx(ph, 2)
                mod = prep_mod(ph, 1, 0, ["g1", "G2", "S2"], ncx)
                xr = kb.ring(ph, 2, [128, D], F32, "xG")
                uwr = kb.ring(ph, 2, [128, 8, 158], BF16, "uw")
                vr = kb.ring(ph, 1, [128, D], F32, "vG")
                bnr = kb.ring(ph, 2, [128, 16], F32, "bnG")
                nr = kb.ring(ph, 1, [128, D], F32, "nG")
                sr = kb.ring(ph, 2, [128, D], BF16, "sG")
                sTr = kb.ring(ph, 2, [128, 8, 128], BF16, "sTG")
                def ld_g(i):
                    xt = xr.next()
                    kb.dma(SP, xt.h[:], X2[i * 128:(i + 1) * 128, :], w=[xt.d])
                    uw = uwr.next()
                    t0 = (i - 1) * 128 - 15
                    kb.dma(SP, uw.h[:], UTs[:, :, t0:t0 + 158], w=[uw.d])
                    return xt, uw

                pfx = Prefetch(OWN, ld_g, 1)
                for ki, i in enumerate(OWN):
                    xt, uw = pfx.get(ki)
                    pc = [psf.next(), psf.next()]

                    def f(pc=pc, uw=uw):
                        ins = None
                        for c in range(8):
                            for j in range(31):
                                ins = nc_t.matmul(pc[c // 4].h[:, (c % 4) * 128:(c % 4 + 1) * 128],
                                                  lhsT=uw.h[:, c, j:j + 128], rhs=DG.h[:, c, j, :],
                                                  start=(j == 0), stop=(j == 30))
                        return ins

                    kb.op(PE, f, r=[uw.d] + DGd, w=[pc[0].d, pc[1].d])
                    v = vr.next()
                    bn = bnr.next()
                    for hf in range(2):
                        kb.op(DVE, lambda v=v, pc=pc, hf=hf: nc_v.tensor_tensor(
                            out=v.h[:, hf * 512:(hf + 1) * 512], in0=pc[hf].h[:], in1=dwb.h[:, hf * 512:(hf + 1) * 512],
                            op=ALU.add), r=[pc[hf].d, dwb.d], w=[v.d])
                    for hf in range(2):
                        kb.op(DVE, lambda v=v, bn=bn, hf=hf: nc_v.bn_stats(out=bn.h[:, hf * 6:(hf + 1) * 6],
                                                                           in_=v.h[:, hf * 512:(hf + 1) * 512]),
                              r=[v.d], w=[bn.d])
                    kb.op(DVE, lambda bn=bn: nc_v.bn_aggr(out=bn.h[:, 12:14], in_=bn.h[:, 0:12]), w=[bn.d])
                    kb.op(ACT, lambda bn=bn: nc_s.activation(out=bn.h[:, 14:15], in_=bn.h[:, 13:14], func=AF.Sqrt,
                                                             bias=epst.h[:, 0:1], scale=1.0), r=[epst.d], w=[bn.d])
                    kb.op(DVE, lambda bn=bn: nc_v.reciprocal(out=bn.h[:, 15:16], in_=bn.h[:, 14:15]), w=[bn.d])
                    n_ = nr.next()
                    kb.op(DVE, lambda n_=n_, v=v, bn=bn: nc_v.tensor_scalar(
                        out=n_.h[:], in0=v.h[:], scalar1=bn.h[:, 12:13], scalar2=bn.h[:, 15:16], op0=ALU.subtract,
                        op1=ALU.mult), r=[v.d, bn.d], w=[n_.d])
                    kb.op(POOL, lambda n_=n_: nc_g.tensor_tensor(out=n_.h[:], in0=n_.h[:], in1=lng.h[:], op=ALU.mult),
                          r=[lng.d], w=[n_.d])
                    kb.op(POOL, lambda n_=n_: nc_g.tensor_tensor(out=n_.h[:], in0=n_.h[:], in1=lnb.h[:], op=ALU.add),
                          r=[lnb.d], w=[n_.d])
                    s_ = sr.next()
                    kb.op(ACT, lambda s_=s_, n_=n_: nc_s.activation(out=s_.h[:], in_=n_.h[:], func=AF.Silu),
                          r=[n_.d], w=[s_.d])
                    sT = sTr.next()
                    transposes_bf(psb, [s_.h[:, k * 128:(k + 1) * 128] for k in range(8)], [s_.d],
                                  sT.h[:].rearrange("p k t -> p (k t)"), [sT.d], ACT)
                    pm = [psf.next(), psf.next()]
                    for c in range(2):
                        linear(pm[c].h[:], pm[c].d, sT, Wpw2, c * 512, 512)
                    post.run(i, pm, b2, xt, mod, ncx, X3, FTs, wts[1])
                post.routing(wts[1])
            if done("G"):
                return nc, phases_done
            convsc.close()

            moe_phase(kb, nc, 1, OWN, MOD[1, 0:1, 5 * D:6 * D], X3, None, FTs, wts[1], moe_g, moe_u, moe_d, out, fin_g, epst)
        kb.barrier()
        phases_done.append("H")
    return nc, phases_done


class PostMix:
    def __init__(self, kb, nc, scope, identf, epst, depth=2):
        self.kb, self.nc, self.scope, self.identf, self.epst = kb, nc, scope, identf, epst
        self.depth = depth

    def setup(self, router_w, router_b, psf):
        kb, nc, ph = self.kb, self.nc, self.scope
        self.psf = psf
        self.rw = kb.sb(ph, [128, 8, NE], F32, "rw")
        kb.dma(kb.SP, self.rw.h[:], router_w.rearrange("(k p) n -> p k n", p=128), w=[self.rw.d])
        self.rb = kb.sb(ph, [128, NE], F32, "rb")
        kb.dma(kb.SP, self.rb.h[:], router_b.partition_broadcast(128), w=[self.rb.d])
        self.sc_all = kb.sb(ph, [128, NT, NE], F32, "sc_all")
        kb.op(kb.POOL, lambda: nc.gpsimd.memset(self.sc_all.h[:], 0.5), w=[self.sc_all.d])
        self.tmpr = kb.ring(ph, self.depth, [128, D], F32, "pmtmp")
        self.x1r = kb.ring(ph, self.depth, [128, D], F32, "pmx1")
        self.fr = kb.ring(ph, self.depth, [128, D], F32, "pmf")
        self.fTr = kb.ring(ph, self.depth, [128, 8, 128], F32, "pmfT")
        self.fbr = kb.ring(ph, self.depth, [128, 8, 128], BF16, "pmfb")

    def run(self, i, pm, bias, xt, mod, ncx, Xout, FTs, wts):
        kb, nc = self.kb, self.nc
        nc_v, nc_s, nc_g, nc_t = nc.vector, nc.scalar, nc.gpsimd, nc.tensor
        DVE, POOL, ACT, PE, SP = kb.DVE, kb.POOL, kb.ACT, kb.PE, kb.SP
        tmp = self.tmpr.next()
        x1 = self.x1r.next()
        for c in range(2):
            sl = slice(c * 512, (c + 1) * 512)
            if bias is not None:
                kb.op(DVE, lambda c=c, sl=sl: nc_v.tensor_tensor(out=tmp.h[:, sl], in0=pm[c].h[:], in1=bias.h[:, sl],
                                                                 op=ALU.add), r=[pm[c].d, bias.d], w=[tmp.d])
                kb.op(POOL, lambda sl=sl: nc_g.tensor_tensor(out=tmp.h[:, sl], in0=tmp.h[:, sl], in1=mod["g1"].h[:, sl],
                                                             op=ALU.mult), r=[mod["g1"].d], w=[tmp.d])
            else:
                kb.op(DVE, lambda c=c, sl=sl: nc_v.tensor_tensor(out=tmp.h[:, sl], in0=pm[c].h[:],
                                                                 in1=mod["g1"].h[:, sl], op=ALU.mult),
                      r=[pm[c].d, mod["g1"].d], w=[tmp.d])
        kb.op(POOL, lambda: nc_g.tensor_tensor(out=x1.h[:], in0=tmp.h[:], in1=xt.h[:], op=ALU.add),
              r=[tmp.d, xt.d], w=[x1.d])
        kb.dma(SP, Xout[i * 128:(i + 1) * 128, :], x1.h[:], r=[x1.d])
        f = self.fr.next()
        junk = ncx.junk.next()
        st = ncx.stat.next()
        kb.op(ACT, lambda: nc_s.activation(out=junk.h[:], in_=x1.h[:], func=AF.Square, scale=1.0 / 32.0,
                                           accum_out=st.h[:, 0:1]), r=[x1.d], w=[junk.d, st.d])
        kb.op(ACT, lambda: nc_s.activation(out=st.h[:, 1:2], in_=st.h[:, 0:1], func=AF.Sqrt,
                                           bias=self.epst.h[:, 0:1], scale=1.0), r=[self.epst.d], w=[st.d])
        kb.op(DVE, lambda: nc_v.reciprocal(out=st.h[:, 2:3], in_=st.h[:, 1:2]), w=[st.d])
        t2 = ncx.tmp.next()
        kb.op(DVE, lambda: nc_v.scalar_tensor_tensor(out=t2.h[:], in0=x1.h[:], scalar=st.h[:, 2:3], in1=mod["G2"].h[:],
                                                     op0=ALU.mult, op1=ALU.mult), r=[x1.d, st.d, mod["G2"].d], w=[t2.d])
        kb.op(POOL, lambda: nc_g.tensor_tensor(out=f.h[:], in0=t2.h[:], in1=mod["S2"].h[:], op=ALU.add),
              r=[t2.d, mod["S2"].d], w=[f.d])
        fT = self.fTr.next()
        for hf in range(2):
            ps = self.psf.next()

            def g(ps=ps, hf=hf):
                ins = None
                for k in range(4):
                    kk = hf * 4 + k
                    ins = nc_t.transpose(out=ps.h[:, k * 128:(k + 1) * 128], in_=f.h[:, kk * 128:(kk + 1) * 128],
                                         identity=self.identf.h[:])
                return ins

            kb.op(PE, g, r=[f.d, self.identf.d], w=[ps.d])
            kb.op(ACT, lambda ps=ps, hf=hf: nc_s.copy(out=fT.h[:, hf * 4:(hf + 1) * 4, :].rearrange("p k t -> p (k t)"),
                                                      in_=ps.h[:]), r=[ps.d], w=[fT.d])
        fb = self.fbr.next()
        kb.op(POOL, lambda: nc_g.tensor_copy(out=fb.h[:], in_=fT.h[:]), r=[fT.d], w=[fb.d])
        kb.dma(SP, FTs[i], fb.h[:].rearrange("p k t -> p (k t)"), r=[fb.d])
        psl = self.psf.next()

        def g2():
            ins = None
            for k in range(8):
                ins = nc_t.matmul(psl.h[:, 0:NE], lhsT=fT.h[:, k, :], rhs=self.rw.h[:, k, :], start=(k == 0), stop=(k == 7))
            return ins

        kb.op(PE, g2, r=[fT.d, self.rw.d], w=[psl.d])
        kb.op(ACT, lambda: nc_s.activation(out=self.sc_all.h[:, i, :], in_=psl.h[:, 0:NE], func=AF.Sigmoid),
              r=[psl.d], w=[self.sc_all.d])

    def routing(self, wts):
        kb, nc, ph = self.kb, self.nc, self.scope
        nc_v = nc.vector
        DVE = kb.DVE
        T = NT
        sc = self.sc_all
        bi = kb.sb(ph, [128, T, NE], F32, "r_bi")
        b2 = kb.sb(ph, [128, T, NE], F32, "r_b2")
        e1 = kb.sb(ph, [128, T, NE], F32, "r_e1")
        e2 = kb.sb(ph, [128, T, NE], F32, "r_e2")
        m1 = kb.sb(ph, [128, T, 4], F32, "r_m1")
        m2 = kb.sb(ph, [128, T, 4], F32, "r_m2")
        gs = kb.sb(ph, [128, T, 4], F32, "r_gs")
        gm = kb.sb(ph, [128, T], F32, "r_gm")
        ws = kb.sb(ph, [128, T], F32, "r_ws")
        v4 = lambda t: t.h[:].rearrange("p t (g e) -> p t g e", g=4)
        bc4 = lambda t: t.h[:].unsqueeze(3).broadcast_to([128, T, 4, 4])
        kb.op(DVE, lambda: nc_v.tensor_tensor(out=bi.h[:], in0=sc.h[:], in1=self.rb.h[:].unsqueeze(1).broadcast_to([128, T, NE]),
                                              op=ALU.add), r=[sc.d, self.rb.d], w=[bi.d])
        kb.op(DVE, lambda: nc_v.tensor_reduce(out=m1.h[:], in_=v4(bi), axis=AX.X, op=ALU.max), r=[bi.d], w=[m1.d])
        kb.op(DVE, lambda: nc_v.tensor_tensor(out=v4(e1), in0=v4(bi), in1=bc4(m1), op=ALU.is_equal), r=[bi.d, m1.d], w=[e1.d])
        kb.op(DVE, lambda: nc_v.scalar_tensor_tensor(out=b2.h[:], in0=e1.h[:], scalar=-1e9, in1=bi.h[:], op0=ALU.mult,
                                                     op1=ALU.add), r=[e1.d, bi.d], w=[b2.d])
        kb.op(DVE, lambda: nc_v.tensor_reduce(out=m2.h[:], in_=v4(b2), axis=AX.X, op=ALU.max), r=[b2.d], w=[m2.d])
        kb.op(DVE, lambda: nc_v.tensor_tensor(out=gs.h[:], in0=m1.h[:], in1=m2.h[:], op=ALU.add), r=[m1.d, m2.d], w=[gs.d])
        kb.op(DVE, lambda: nc_v.tensor_reduce(out=gm.h[:], in_=gs.h[:], axis=AX.X, op=ALU.max), r=[gs.d], w=[gm.d])
        kb.op(DVE, lambda: nc_v.tensor_tensor(out=gs.h[:], in0=gs.h[:], in1=gm.h[:].unsqueeze(2).broadcast_to([128, T, 4]),
                                              op=ALU.is_equal), r=[gm.d], w=[gs.d])
        kb.op(DVE, lambda: nc_v.tensor_tensor(out=v4(e2), in0=v4(b2), in1=bc4(m2), op=ALU.is_equal), r=[b2.d, m2.d], w=[e2.d])
        kb.op(DVE, lambda: nc_v.tensor_tensor(out=e1.h[:], in0=e1.h[:], in1=e2.h[:], op=ALU.add), r=[e2.d], w=[e1.d])
        kb.op(DVE, lambda: nc_v.tensor_tensor(out=v4(e1), in0=v4(e1), in1=bc4(gs), op=ALU.mult), r=[gs.d], w=[e1.d])
        kb.op(DVE, lambda: nc_v.tensor_tensor(out=e1.h[:], in0=e1.h[:], in1=sc.h[:], op=ALU.mult), r=[sc.d], w=[e1.d])
        kb.op(DVE, lambda: nc_v.tensor_reduce(out=ws.h[:], in_=e1.h[:], axis=AX.X, op=ALU.add), r=[e1.d], w=[ws.d])
        kb.op(DVE, lambda: nc_v.reciprocal(out=ws.h[:], in_=ws.h[:]), w=[ws.d])
        kb.op(DVE, lambda: nc_v.tensor_tensor(out=wts.h[:], in0=e1.h[:], in1=ws.h[:].unsqueeze(2).broadcast_to([128, T, NE]),
                                              op=ALU.mult), r=[e1.d, ws.d], w=[wts.d])


def moe_phase(kb, nc, li, tiles, gate2_row, Xin, Xout, FTs, wts, moe_g, moe_u, moe_d, out, fin_g, epst):
    nc_v, nc_s, nc_g, nc_t = nc.vector, nc.scalar, nc.gpsimd, nc.tensor
    PE, ACT, DVE, POOL, SP = kb.PE, kb.ACT, kb.DVE, kb.POOL, kb.SP
    GMAX = 12
    groups = [tiles[a:a + GMAX] for a in range(0, len(tiles), GMAX)]
    with contextlib.ExitStack() as ph:
        fTgr = kb.ring(ph, 2, [128, GMAX, 8, 128], BF16, "fTg")
        yacc = kb.sb(ph, [128, GMAX, D], F32, "yacc")
        yd = [Dep() for _ in range(GMAX)]
        wgr = kb.ring(ph, 2, [128, 8, FF], BF16, "Wg")
        wur = kb.ring(ph, 2, [128, 8, FF], BF16, "Wu")
        wdr = kb.ring(ph, 2, [128, 4, D], BF16, "Wd")
        psgu = kb.ring(ph, 4, [128, 512], F32, "psgu", psum=True)
        psy = kb.ring(ph, 4, [128, 512], F32, "psy", psum=True)
        sgr = kb.ring(ph, 2, [128, 512], BF16, "sgt")
        hidr = kb.ring(ph, 2, [128, 4, 512], BF16, "hid")
        xr = kb.ring(ph, 2, [128, D], F32, "xE")
        tr = kb.ring(ph, 2, [128, D], F32, "tE")
        orr = kb.ring(ph, 2, [128, D], F32, "oE")
        gate2 = kb.sb(ph, [128, D], F32, "gate2")
        kb.dma(SP, gate2.h[:], gate2_row.partition_broadcast(128), w=[gate2.d])
        if out is not None:
            fg = kb.sb(ph, [128, D], F32, "fing")
            kb.dma(SP, fg.h[:], fin_g.partition_broadcast(128), w=[fg.d])
            junkr = kb.ring(ph, 2, [128, D], BF16, "junkE")
            str_ = kb.ring(ph, 2, [128, 4], F32, "stE")
        def ld_ft(grp):
            nt = len(grp)
            assert nt % 4 == 0 and grp == list(range(grp[0], grp[0] + nt))
            ft = fTgr.next()
            kb.dma(SP, ft.h[:, 0:nt, :, :].rearrange("p n k t -> p n (k t)"),
                   FTs[grp[0]:grp[0] + nt].rearrange("n p f -> p n f"), w=[ft.d])
            return ft

        pft = Prefetch(groups, ld_ft, 1)
        for gi, grp in enumerate(groups):
            nt = len(grp)
            fTg = pft.get(gi)
            for e in range(NE):
                Wg, Wu, Wd = wgr.next(), wur.next(), wdr.next()
                kb.dma(POOL, Wg.h[:], moe_g[li, e].rearrange("(k p) n -> p k n", p=128), w=[Wg.d])
                kb.dma(POOL, Wu.h[:], moe_u[li, e].rearrange("(k p) n -> p k n", p=128), w=[Wu.d])
                kb.dma(POOL, Wd.h[:], moe_d[li, e].rearrange("(k p) n -> p k n", p=128), w=[Wd.d])
                for sgi in range(nt // 4):
                    hid = hidr.next()
                    for j in range(4):
                        pg = psgu.next()
                        pu = psgu.next()

                        def f(pg=pg, pu=pu, j=j, sgi=sgi, Wg=Wg, Wu=Wu):
                            ins = None
                            for k in range(8):
                                nc_t.matmul(pg.h[:], lhsT=Wg.h[:, k, j * 128:(j + 1) * 128],
                                            rhs=fTg.h[:, sgi * 4:(sgi + 1) * 4, k, :], start=(k == 0), stop=(k == 7))
                            for k in range(8):
                                ins = nc_t.matmul(pu.h[:], lhsT=Wu.h[:, k, j * 128:(j + 1) * 128],
                                                  rhs=fTg.h[:, sgi * 4:(sgi + 1) * 4, k, :], start=(k == 0), stop=(k == 7))
                            return ins

                        kb.op(PE, f, r=[Wg.d, Wu.d, fTg.d], w=[pg.d, pu.d])
                        sg = sgr.next()
                        kb.op(ACT, lambda sg=sg, pg=pg: nc_s.activation(out=sg.h[:], in_=pg.h[:], func=AF.Silu),
                              r=[pg.d], w=[sg.d])
                        kb.op(DVE, lambda hid=hid, j=j, sg=sg, pu=pu: nc_v.tensor_tensor(out=hid.h[:, j, :], in0=pu.h[:],
                                                                                         in1=sg.h[:], op=ALU.mult),
                              r=[pu.d, sg.d], w=[hid.d])
                    for t in range(4):
                        ti = sgi * 4 + t
                        tile_idx = grp[ti]
                        for c in range(2):
                            py = psy.next()

                            def f2(py=py, hid=hid, t=t, c=c, Wd=Wd):
                                ins = None
                                for j in range(4):
                                    ins = nc_t.matmul(py.h[:], lhsT=hid.h[:, j, t * 128:(t + 1) * 128],
                                                      rhs=Wd.h[:, j, c * 512:(c + 1) * 512], start=(j == 0), stop=(j == 3))
                                return ins

                            kb.op(PE, f2, r=[hid.d, Wd.d], w=[py.d])
                            if e == 0:
                                kb.op(DVE, lambda py=py, ti=ti, c=c, tile_idx=tile_idx, e=e: nc_v.tensor_scalar(
                                    out=yacc.h[:, ti, c * 512:(c + 1) * 512], in0=py.h[:],
                                    scalar1=wts.h[:, tile_idx, e:e + 1], scalar2=None, op0=ALU.mult),
                                      r=[py.d, wts.d], w=[yd[ti]])
                            else:
                                kb.op(DVE, lambda py=py, ti=ti, c=c, tile_idx=tile_idx, e=e: nc_v.scalar_tensor_tensor(
                                    out=yacc.h[:, ti, c * 512:(c + 1) * 512], in0=py.h[:], scalar=wts.h[:, tile_idx, e:e + 1],
                                    in1=yacc.h[:, ti, c * 512:(c + 1) * 512], op0=ALU.mult, op1=ALU.add),
                                      r=[py.d, wts.d], w=[yd[ti]])
            for ti, tile_idx in enumerate(grp):
                xt = xr.next()
                kb.dma(SP, xt.h[:], Xin[tile_idx * 128:(tile_idx + 1) * 128, :], w=[xt.d])
                tt = tr.next()
                kb.op(DVE, lambda tt=tt, ti=ti: nc_v.tensor_tensor(out=tt.h[:], in0=yacc.h[:, ti, :], in1=gate2.h[:],
                                                                   op=ALU.mult), r=[yd[ti], gate2.d], w=[tt.d])
                ot = orr.next()
                kb.op(POOL, lambda ot=ot, tt=tt, xt=xt: nc_g.tensor_tensor(out=ot.h[:], in0=tt.h[:], in1=xt.h[:], op=ALU.add),
                      r=[tt.d, xt.d], w=[ot.d])
                if out is None:
                    kb.dma(SP, Xout[tile_idx * 128:(tile_idx + 1) * 128, :], ot.h[:], r=[ot.d])
                else:
                    junk = junkr.next()
                    st = str_.next()
                    kb.op(ACT, lambda junk=junk, st=st, ot=ot: nc_s.activation(out=junk.h[:], in_=ot.h[:], func=AF.Square,
                                                                               scale=1.0 / 32.0, accum_out=st.h[:, 0:1]),
                          r=[ot.d], w=[junk.d, st.d])
                    kb.op(ACT, lambda st=st: nc_s.activation(out=st.h[:, 1:2], in_=st.h[:, 0:1], func=AF.Sqrt,
                                                             bias=epst.h[:, 0:1], scale=1.0), r=[epst.d], w=[st.d])
                    kb.op(DVE, lambda st=st: nc_v.reciprocal(out=st.h[:, 2:3], in_=st.h[:, 1:2]), w=[st.d])
                    o2 = tr.next()
                    kb.op(DVE, lambda o2=o2, ot=ot, st=st: nc_v.scalar_tensor_tensor(
                        out=o2.h[:], in0=ot.h[:], scalar=st.h[:, 2:3], in1=fg.h[:], op0=ALU.mult, op1=ALU.mult),
                          r=[ot.d, st.d, fg.d], w=[o2.d])
                    oi = tile_idx - 2
                    kb.dma(SP, out[oi * 128:(oi + 1) * 128, :], o2.h[:], r=[o2.d])


def _const_tables():
    t = {}
    t["c_ident"] = np.eye(128, dtype=np.float32)
    c = np.arange(128)
    ang = 2 * np.pi * np.outer(c, c) / 128.0
    t["c_fc"] = (np.concatenate([np.cos(ang), -np.sin(ang)], axis=1) / 1024.0).astype(np.float32)
    t["c_f128"] = np.stack([np.cos(ang), np.sin(ang), -np.sin(ang)]).astype(np.float32)
    k1 = np.arange(128)[:, None]
    l2 = np.arange(64)[None, :]
    a = 2 * np.pi * k1 * l2 / 8192.0
    t["c_tw"] = np.stack([np.cos(a), -np.sin(a)], axis=1).astype(np.float32)
    kk = np.arange(128)[:, None]
    qq = np.arange(128)[None, :]
    mp = (kk >= qq).astype(np.float32)
    mn = (kk <= qq).astype(np.float32)
    t["c_mask"] = np.stack([np.tile(mp, (1, 4)), np.tile(mn, (1, 4))]).astype(np.float32)
    return t


def _core_tables(s):
    t = {}
    g = 32 * s - 2 + np.arange(NT)
    valid = (g >= 0) & (g < 64)
    l2 = np.arange(64)[:, None]
    a = 2 * np.pi * l2 * g[None, :] / 64.0
    f64 = np.stack([np.cos(a), np.sin(a)]) * valid[None, None, :]
    t["c_f64"] = f64.astype(np.float32)
    tv = np.ones((128, NTK), np.float32)
    tv[:, :NT] = valid[None, :].astype(np.float32)
    t["c_tvalid"] = tv
    pos = (g[:, None] * 128 + np.arange(128)[None, :]).reshape(-1).astype(np.float64)
    pos = np.clip(pos, 0, SEQ - 1)
    row = np.floor(pos / 64.0)
    col = pos - row * 64.0
    inv = 10000.0 ** (-np.arange(16, dtype=np.float64) / 16.0)
    ang = np.stack([row[:, None] * inv[None, :], col[:, None] * inv[None, :]], axis=1)
    cos = np.tile(np.cos(ang)[:, None, :, :], (1, 8, 1, 1)).reshape(NT * 128, 256)
    sin = np.tile(np.sin(ang)[:, None, :, :], (1, 8, 1, 1)).reshape(NT * 128, 256)
    cos = np.concatenate([cos, np.ones((256, 256))], axis=0)
    sin = np.concatenate([sin, np.zeros((256, 256))], axis=0)
    t["c_cos"] = cos.astype(np.float32)
    t["c_sin"] = sin.astype(np.float32)
    return t


def make_in_maps(inputs, cores=None):
    f = lambda a: np.ascontiguousarray(np.asarray(a, dtype=np.float32))
    x = f(inputs["x"])
    c = f(inputs["c"])
    ctx = f(inputs["ctx"])
    c_ctx = f(inputs["c_ctx"])
    shared = {
        "ada_w": f(inputs["ada_w"]), "ada_b": f(inputs["ada_b"]),
        "norm_mix_g": f(inputs["norm_mix_g"]), "norm_ffn_g": f(inputs["norm_ffn_g"]),
        "final_norm_g": f(inputs["final_norm_g"]).reshape(1, D),
        "w_in": f(inputs["even_w_in"][0]), "w_in_fT": f(np.asarray(inputs["even_w_in"][0])[:, :512].T),
        "w_out": f(inputs["even_w_out"][0]), "sink": f(inputs["even_sink"]).reshape(1, 8),
        "pw1_w": f(inputs["conv_pw1_w"][0]), "pw1_b": f(inputs["conv_pw1_b"]).reshape(1, 2 * D),
        "dw_wT": f(np.asarray(inputs["conv_dw_w"][0]).T), "dw_b": f(inputs["conv_dw_b"]).reshape(1, D),
        "ln_g": f(inputs["conv_ln_g"]).reshape(1, D), "ln_b": f(inputs["conv_ln_b"]).reshape(1, D),
        "pw2_w": f(inputs["conv_pw2_w"][0]), "pw2_b": f(inputs["conv_pw2_b"]).reshape(1, D),
        "router_w": f(inputs["router_w"]), "router_b": f(inputs["router_b"]).reshape(1, NE),
        "moe_w_gate": f(inputs["moe_w_gate"]), "moe_w_up": f(inputs["moe_w_up"]), "moe_w_down": f(inputs["moe_w_down"]),
    }
    shared.update(_const_tables())
    ctabs = [_core_tables(0), _core_tables(1)]
    maps = []
    for core in (cores if cores is not None else range(8)):
        b, s = core // 2, core % 2
        m = dict(shared)
        m.update(ctabs[s])
        xp = np.zeros((NT * 128, D), np.float32)
        g0 = 32 * s - 2
        lo, hi = max(g0, 0), min(g0 + NT, 64)
        xp[(lo - g0) * 128:(hi - g0) * 128] = x[b, lo * 128:hi * 128]
        m["xw"] = xp
        m["xfull"] = x[b]
        m["ctxb"] = ctx[b]
        cv = np.stack([c[b], c_ctx], axis=0)
        m["cvecT"] = np.ascontiguousarray(cv.reshape(2, 8, 128).transpose(2, 1, 0))
        maps.append(m)
    return maps


_PROGRAM = None


def kernel(**inputs):
    global _PROGRAM
    if _PROGRAM is None:
        _PROGRAM = build_program()[0]
    maps = make_in_maps(inputs)
    res = run_bass_kernel_spmd(_PROGRAM, maps, core_ids=list(range(8)))
    outp = np.empty((4, SEQ, D), np.float32)
    for core in range(8):
        b, s = core // 2, core % 2
        outp[b, s * 4096:(s + 1) * 4096] = np.asarray(res.results[core]["out"]).reshape(4096, D)
    return outp
```

```python
import contextlib
import math
import numpy as np
import concourse.bass as bass
import concourse.mybir as mybir
from concourse.bass_utils import run_bass_kernel_spmd

F32 = mybir.dt.float32
BF16 = mybir.dt.bfloat16
AF = mybir.ActivationFunctionType
ALU = mybir.AluOpType
AX = mybir.AxisListType

D = 1024
SEQ = 8192
NT = 36
NTK = 38
L0 = list(range(1, 35))
OWN = list(range(2, 34))
EPS = 1e-6
NE = 16
FF = 512


class Dep:
    __slots__ = ("w", "r")

    def __init__(self):
        self.w = {}
        self.r = {}


class Tl:
    def __init__(self, h):
        self.h = h
        self.d = Dep()


class Ring:
    def __init__(self, tiles):
        self.t = tiles
        self.i = 0

    def next(self):
        t = self.t[self.i % len(self.t)]
        self.i += 1
        return t


class Prefetch:
    def __init__(self, items, load_fn, pf):
        self.items = list(items)
        self.load = load_fn
        self.pf = pf
        self.q = {}
        self.nxt = 0

    def get(self, k):
        while self.nxt < len(self.items) and self.nxt <= k + self.pf:
            self.q[self.nxt] = self.load(self.items[self.nxt])
            self.nxt += 1
        return self.q.pop(k)


class Eng:
    def __init__(self, name, eng, sem, is_pe=False):
        self.name = name
        self.eng = eng
        self.sem = sem
        self.cnt = 0
        self.known = {}
        self.is_pe = is_pe
        self.dma_sems = []
        self.dma_tot = []
        self.rr = 0


class KB:
    def __init__(self, nc, es):
        self.nc = nc
        self.es = es
        self.uid = 0
        mk = lambda n: es.enter_context(nc.semaphore(n))
        self.PE = Eng("pe", nc.tensor, mk("s_pe"), True)
        self.ACT = Eng("act", nc.scalar, mk("s_act"))
        self.DVE = Eng("dve", nc.vector, mk("s_dve"))
        self.POOL = Eng("pool", nc.gpsimd, mk("s_pool"))
        self.SP = Eng("sp", nc.sync, mk("s_sp"))
        for Q, n in ((self.SP, 32), (self.POOL, 24)):
            Q.dma_sems = [mk(f"d_{Q.name}{i}") for i in range(n)]
            Q.dma_tot = [0] * n
        self.bar_sem = mk("s_bar")
        self.bar_cnt = 0
        self.engs = [self.PE, self.ACT, self.DVE, self.POOL, self.SP]

    def sb(self, scope, shape, dtype, name="t"):
        self.uid += 1
        return Tl(scope.enter_context(self.nc.sbuf_tensor(f"{name}_{self.uid}", list(shape), dtype)))

    def ps(self, scope, shape, dtype, name="p"):
        self.uid += 1
        return Tl(scope.enter_context(self.nc.psum_tensor(f"{name}_{self.uid}", list(shape), dtype)))

    def ring(self, scope, n, shape, dtype, name="r", psum=False):
        f = self.ps if psum else self.sb
        return Ring([f(scope, shape, dtype, name) for _ in range(n)])

    def _need(self, E, r, w):
        need = {}

        def add(dd):
            for key, sv in dd.items():
                if key not in need or need[key][1] < sv[1]:
                    need[key] = sv

        for d in r:
            add(d.w)
        for d in w:
            add(d.w)
            add(d.r)
        for key, (sem, val) in need.items():
            if E.is_pe and key == E.name:
                continue
            if E.known.get(key, 0) < val:
                E.eng.wait_ge(sem, val)
                E.known[key] = val

    @staticmethod
    def _record(key, ev, r, w):
        for d in r:
            d.r[key] = ev
        for d in w:
            d.w = {key: ev}
            d.r = {}

    def op(self, E, fn, r=(), w=()):
        self._need(E, r, w)
        ins = fn()
        E.cnt += 1
        ins.then_inc(E.sem, 1)
        self._record(E.name, (E.sem, E.cnt), r, w)

    def dma(self, Q, out_ap, in_ap, r=(), w=()):
        self._need(Q, r, w)
        k = Q.rr
        Q.rr = (k + 1) % len(Q.dma_sems)
        sem = Q.dma_sems[k]
        key = f"{Q.name}_d{k}"
        if Q.known.get(key, 0) < Q.dma_tot[k]:
            Q.eng.wait_ge(sem, Q.dma_tot[k])
            Q.known[key] = Q.dma_tot[k]
        Q.eng.dma_start(out=out_ap, in_=in_ap).then_inc(sem, 16)
        Q.dma_tot[k] += 16
        self._record(key, (sem, Q.dma_tot[k]), r, w)

    def barrier(self):
        SP = self.SP
        for E in self.engs:
            if E is SP:
                continue
            if SP.known.get(E.name, 0) < E.cnt:
                SP.eng.wait_ge(E.sem, E.cnt)
                SP.known[E.name] = E.cnt
        for Q in (self.SP, self.POOL):
            for k, sem in enumerate(Q.dma_sems):
                key = f"{Q.name}_d{k}"
                if SP.known.get(key, 0) < Q.dma_tot[k]:
                    SP.eng.wait_ge(sem, Q.dma_tot[k])
                    SP.known[key] = Q.dma_tot[k]
        self.bar_cnt += 1
        SP.eng.sem_inc(self.bar_sem, 1)
        for E in self.engs:
            if E is SP:
                continue
            E.eng.wait_ge(self.bar_sem, self.bar_cnt)
            for F in self.engs:
                E.known[F.name] = F.cnt
            for Q in (self.SP, self.POOL):
                for k in range(len(Q.dma_sems)):
                    E.known[f"{Q.name}_d{k}"] = Q.dma_tot[k]


def build_program(stop_after=None, debug=False):
    nc = bass.Bass("TRN2", target_bir_lowering=False)
    nc_v, nc_s, nc_g, nc_t = nc.vector, nc.scalar, nc.gpsimd, nc.tensor

    def din(name, shape, dt=F32):
        return nc.dram_tensor(name, list(shape), dt, kind="ExternalInput").ap()

    dbg = set(debug) if debug else set()

    def dscr(name, shape, dt):
        return nc.dram_tensor(name, list(shape), dt, kind=("ExternalOutput" if name in dbg else "Internal")).ap()

    xw = din("xw", [NT * 128, D])
    xfull = din("xfull", [SEQ, D])
    ctxb = din("ctxb", [256, D])
    cvecT = din("cvecT", [128, 8, 2])
    ada_w = din("ada_w", [2, D, 6 * D])
    ada_b = din("ada_b", [2, 6 * D])
    nmix_g = din("norm_mix_g", [2, D])
    nffn_g = din("norm_ffn_g", [2, D])
    fin_g = din("final_norm_g", [1, D])
    w_in = din("w_in", [D, 1280])
    w_in_fT = din("w_in_fT", [512, D])
    w_out = din("w_out", [D, D])
    sink = din("sink", [1, 8])
    pw1_w = din("pw1_w", [D, 2 * D])
    pw1_b = din("pw1_b", [1, 2 * D])
    dw_wT = din("dw_wT", [D, 31])
    dw_b = din("dw_b", [1, D])
    ln_g = din("ln_g", [1, D])
    ln_b = din("ln_b", [1, D])
    pw2_w = din("pw2_w", [D, D])
    pw2_b = din("pw2_b", [1, D])
    router_w = din("router_w", [D, NE])
    router_b = din("router_b", [1, NE])
    need_moe = stop_after is None or stop_after >= "E"
    moe_g = din("moe_w_gate", [2, NE, D, FF]) if need_moe else None
    moe_u = din("moe_w_up", [2, NE, D, FF]) if need_moe else None
    moe_d = din("moe_w_down", [2, NE, FF, D]) if need_moe else None
    c_ident = din("c_ident", [128, 128])
    c_fc = din("c_fc", [128, 256])
    c_f128 = din("c_f128", [3, 128, 128])
    c_tw = din("c_tw", [128, 2, 64])
    c_f64 = din("c_f64", [2, 64, NT])
    c_cos = din("c_cos", [NTK * 128, 256])
    c_sin = din("c_sin", [NTK * 128, 256])
    c_mask = din("c_mask", [2, 128, 512])
    c_tvalid = din("c_tvalid", [128, NTK])

    out = nc.dram_tensor("out", [32 * 128, D], F32, kind="ExternalOutput").ap()

    MOD = dscr("s_mod", [2, 2, 6 * D], F32)
    Wf = dscr("s_wf", [SEQ, D], BF16)
    D1 = dscr("s_d1", [128, 64, D], BF16)
    Yf = dscr("s_yf", [NT, 128, 512], BF16)
    QTs = dscr("s_qt", [NT, 64, 1024], BF16)
    X1 = dscr("s_x1", [NT * 128, D], F32)
    FTs = dscr("s_ft", [NT, 128, 1024], BF16)
    X2 = dscr("s_x2", [NT * 128, D], F32)
    UTs = dscr("s_ut", [128, 8, 34 * 128], BF16)
    X3 = dscr("s_x3", [NT * 128, D], F32)

    phases_done = []

    with contextlib.ExitStack() as es:
        kb = KB(nc, es)
        PE, ACT, DVE, POOL, SP = kb.PE, kb.ACT, kb.DVE, kb.POOL, kb.SP

        identb = kb.sb(es, [128, 128], BF16, "identb")
        identf = kb.sb(es, [128, 128], F32, "identf")
        epst = kb.sb(es, [128, 1], F32, "eps")
        tvalid = kb.sb(es, [128, NTK], F32, "tvalid")
        ones2 = kb.sb(es, [128, 2, 1], F32, "ones2")
        wts = [kb.sb(es, [128, NT, NE], F32, f"wts{i}") for i in range(2)]
        kb.dma(POOL, identb.h[:], c_ident, w=[identb.d])
        kb.dma(SP, identf.h[:], c_ident, w=[identf.d])
        kb.dma(SP, tvalid.h[:], c_tvalid, w=[tvalid.d])
        kb.op(POOL, lambda: nc_g.memset(epst.h[:], EPS), w=[epst.d])
        kb.op(POOL, lambda: nc_g.memset(ones2.h[:], 1.0), w=[ones2.d])

        def done(name):
            phases_done.append(name)
            kb.barrier()
            return stop_after == name

        def bc_load(dst, row_ap):
            kb.dma(SP, dst.h[:], row_ap.partition_broadcast(128), w=[dst.d])

        def prep_mod(scope, layer, row, which, ncx):
            res = {}
            m = MOD[layer, row:row + 1, :]
            for nm in which:
                t = kb.sb(scope, [128, D], F32, "mod" + nm)
                if nm in ("G1", "G2"):
                    tmp = ncx.tmp.next()
                    gt = ncx.tmp.next()
                    off = 1 * D if nm == "G1" else 4 * D
                    gsrc = nmix_g if nm == "G1" else nffn_g
                    bc_load(tmp, m[:, off:off + D])
                    bc_load(gt, gsrc[layer:layer + 1, :])
                    kb.op(DVE, lambda t=t, tmp=tmp, gt=gt: nc_v.scalar_tensor_tensor(
                        out=t.h[:], in0=tmp.h[:], scalar=1.0, in1=gt.h[:], op0=ALU.add, op1=ALU.mult),
                          r=[tmp.d, gt.d], w=[t.d])
                else:
                    off = {"S1": 0, "g1": 2 * D, "S2": 3 * D, "g2": 5 * D}[nm]
                    bc_load(t, m[:, off:off + D])
                res[nm] = t
            return res

        class NormCtx:
            def __init__(self, scope, depth=3):
                self.junk = kb.ring(scope, depth, [128, D], BF16, "junk")
                self.stat = kb.ring(scope, 2 * depth, [128, 4], F32, "nstat")
                self.tmp = kb.ring(scope, depth, [128, D], F32, "ntmp")

        def rstd_of(ncx, x_ap, xdeps):
            junk = ncx.junk.next()
            st = ncx.stat.next()
            kb.op(ACT, lambda: nc_s.activation(out=junk.h[:], in_=x_ap, func=AF.Square, scale=1.0 / 32.0,
                                               accum_out=st.h[:, 0:1]), r=xdeps, w=[junk.d, st.d])
            kb.op(ACT, lambda: nc_s.activation(out=st.h[:, 1:2], in_=st.h[:, 0:1], func=AF.Sqrt,
                                               bias=epst.h[:, 0:1], scale=1.0), r=[epst.d], w=[st.d])
            kb.op(DVE, lambda: nc_v.reciprocal(out=st.h[:, 2:3], in_=st.h[:, 1:2]), w=[st.d])
            return st

        def norm_mod(ncx, x_ap, xdeps, G, S, out_ap, outdeps):
            st = rstd_of(ncx, x_ap, xdeps)
            tmp = ncx.tmp.next()
            kb.op(DVE, lambda: nc_v.scalar_tensor_tensor(out=tmp.h[:], in0=x_ap, scalar=st.h[:, 2:3], in1=G.h[:],
                                                         op0=ALU.mult, op1=ALU.mult),
                  r=list(xdeps) + [st.d, G.d], w=[tmp.d])
            kb.op(POOL, lambda: nc_g.tensor_tensor(out=out_ap, in0=tmp.h[:], in1=S.h[:], op=ALU.add),
                  r=[tmp.d, S.d], w=outdeps)

        def transposes_bf(psb_ring, srcs, rdeps, dst_ap, wdeps, evac, rows=128):
            pst = psb_ring.next()
            n = len(srcs)

            def f():
                ins = None
                for k, ap in enumerate(srcs):
                    ins = nc_t.transpose(out=pst.h[0:rows, k * 128:(k + 1) * 128], in_=ap, identity=identb.h[:])
                return ins

            kb.op(PE, f, r=list(rdeps) + [identb.d], w=[pst.d])
            if evac is ACT:
                kb.op(ACT, lambda: nc_s.copy(out=dst_ap, in_=pst.h[0:rows, 0:n * 128]), r=[pst.d], w=wdeps)
            else:
                kb.op(DVE, lambda: nc_v.tensor_copy(out=dst_ap, in_=pst.h[0:rows, 0:n * 128]), r=[pst.d], w=wdeps)

        def linear(ps_ap, psd, hT, W, col0, ncols, extra_r=()):
            def f():
                ins = None
                for k in range(8):
                    ins = nc_t.matmul(ps_ap, lhsT=hT.h[:, k, :], rhs=W.h[:, k, col0:col0 + ncols],
                                      start=(k == 0), stop=(k == 7))
                return ins

            kb.op(PE, f, r=[hT.d, W.d] + list(extra_r), w=[psd])

        def load_w_bf(dst, src_ap):
            kb.dma(POOL, dst.h[:], src_ap.rearrange("(k p) n -> p k n", p=128), w=[dst.d])

        with contextlib.ExitStack() as ph:
            scT = kb.sb(ph, [128, 8, 2], F32, "scT")
            kb.dma(SP, scT.h[:], cvecT, w=[scT.d])
            kb.op(ACT, lambda: nc_s.activation(out=scT.h[:], in_=scT.h[:], func=AF.Silu), w=[scT.d])
            wring = kb.ring(ph, 2, [128, 8, 512], F32, "adaw")
            psr = kb.ring(ph, 2, [128, 512], F32, "psA", psum=True)
            for li in range(2):
                bias = kb.sb(ph, [2, 6 * D], F32, "adab")
                msb = kb.sb(ph, [2, 6 * D], F32, "adam")
                kb.dma(SP, bias.h[:], ada_b[li:li + 1, :].partition_broadcast(2), w=[bias.d])
                for j in range(12):
                    wt = wring.next()
                    kb.dma(SP, wt.h[:], ada_w[li, :, j * 512:(j + 1) * 512].rearrange("(k p) n -> p k n", p=128),
                           w=[wt.d])
                    ps = psr.next()

                    def f(wt=wt, ps=ps):
                        ins = None
                        for k in range(8):
                            ins = nc_t.matmul(ps.h[0:2, :], lhsT=scT.h[:, k, :], rhs=wt.h[:, k, :],
                                              start=(k == 0), stop=(k == 7))
                        return ins

                    kb.op(PE, f, r=[scT.d, wt.d], w=[ps.d])
                    kb.op(DVE, lambda ps=ps, j=j: nc_v.tensor_tensor(out=msb.h[:, j * 512:(j + 1) * 512],
                                                                     in0=ps.h[0:2, :],
                                                                     in1=bias.h[:, j * 512:(j + 1) * 512], op=ALU.add),
                          r=[ps.d, bias.d], w=[msb.d])
                kb.dma(SP, MOD[li], msb.h[:], r=[msb.d])
        if done("A"):
            return nc, phases_done

        with contextlib.ExitStack() as lay:
            with contextlib.ExitStack() as ph:
                Wp = kb.sb(ph, [128, 8, D], BF16, "Wp")
                psf = kb.ring(ph, 6, [128, 512], F32, "psB", psum=True)
                psb = kb.ring(ph, 2, [128, 1024], BF16, "psBb", psum=True)
                with contextlib.ExitStack() as sub:
                    wfT = kb.sb(sub, [128, 4, D], BF16, "wfT")
                    fct = kb.sb(sub, [128, 256], BF16, "fct")
                    kb.dma(POOL, wfT.h[:], w_in_fT.rearrange("(g c) d -> c g d", c=128), w=[wfT.d])
                    kb.dma(POOL, fct.h[:], c_fc, w=[fct.d])
                    for dk in range(8):
                        for gp in range(2):
                            ps = psf.next()

                            def f(ps=ps, dk=dk, gp=gp):
                                ins = None
                                for gg in range(2):
                                    g = gp * 2 + gg
                                    ins = nc_t.matmul(ps.h[:, gg * 256:(gg + 1) * 256],
                                                      lhsT=wfT.h[:, g, dk * 128:(dk + 1) * 128], rhs=fct.h[:],
                                                      start=True, stop=True)
                                return ins

                            kb.op(PE, f, r=[wfT.d, fct.d], w=[ps.d])
                            kb.op(DVE, lambda ps=ps, dk=dk, gp=gp: nc_v.tensor_copy(
                                out=Wp.h[:, dk, gp * 512:(gp + 1) * 512], in_=ps.h[:]), r=[ps.d], w=[Wp.d])
                    kb.barrier()
                    if stop_after == "B0":
                        return nc, phases_done
                with contextlib.ExitStack() as sub:
                    ncx = NormCtx(sub)
                    mod = prep_mod(sub, 0, 0, ["G1", "S1"], ncx)
                    xr = kb.ring(sub, 4, [128, D], F32, "xB")
                    hr = kb.ring(sub, 3, [128, D], BF16, "hB")
                    hTr = kb.ring(sub, 3, [128, 8, 128], BF16, "hTB")
                    wr = kb.ring(sub, 3, [128, D], BF16, "wB")
                    def ld_b1(t):
                        xt = xr.next()
                        kb.dma(SP, xt.h[:], xfull[t * 128:(t + 1) * 128, :], w=[xt.d])
                        return xt

                    pfx = Prefetch(range(64), ld_b1, 2)
                    for t in range(64):
                        xt = pfx.get(t)
                        h = hr.next()
                        norm_mod(ncx, xt.h[:], [xt.d], mod["G1"], mod["S1"], h.h[:], [h.d])
                        hT = hTr.next()
                        transposes_bf(psb, [h.h[:, k * 128:(k + 1) * 128] for k in range(8)], [h.d],
                                      hT.h[:].rearrange("p k t -> p (k t)"), [hT.d], ACT)
                        wt = wr.next()
                        for c in range(2):
                            ps = psf.next()
                            linear(ps.h[:], ps.d, hT, Wp, c * 512, 512)
                            if c == 0:
                                kb.op(ACT, lambda ps=ps, wt=wt: nc_s.copy(out=wt.h[:, 0:512], in_=ps.h[:]),
                                      r=[ps.d], w=[wt.d])
                            else:
                                kb.op(DVE, lambda ps=ps, wt=wt: nc_v.tensor_copy(out=wt.h[:, 512:1024], in_=ps.h[:]),
                                      r=[ps.d], w=[wt.d])
                        kb.dma(SP, Wf[t * 128:(t + 1) * 128, :], wt.h[:], r=[wt.d])
                    kb.barrier()
                    if stop_after == "B1":
                        return nc, phases_done
                with contextlib.ExitStack() as sub:
                    f128 = kb.sb(sub, [128, 3, 128], BF16, "f128")
                    tw = kb.sb(sub, [128, 2, 64], F32, "tw")
                    kb.dma(POOL, f128.h[:], c_f128.rearrange("a p n -> p a n"), w=[f128.d])
                    kb.dma(SP, tw.h[:], c_tw, w=[tw.d])
                    d0r = kb.ring(sub, 4, [128, D], BF16, "d0")
                    d1r = kb.ring(sub, 3, [128, D], BF16, "d1")
                    t1r = kb.ring(sub, 3, [128, 512], F32, "t1")
                    t2r = kb.ring(sub, 3, [128, 512], F32, "t2")
                    Wf_v = Wf.rearrange("(l1 l2) c -> l2 l1 c", l2=64)
                    def ld_b2(l2):
                        d0 = d0r.next()
                        kb.dma(SP, d0.h[:], Wf_v[l2], w=[d0.d])
                        return d0

                    pfx = Prefetch(range(64), ld_b2, 2)
                    for l2 in range(64):
                        d0 = pfx.get(l2)
                        dv = d0.h[:].rearrange("p (g r c) -> p g r c", g=4, r=2)
                        Dr = dv[:, :, 0, :]
                        Di = dv[:, :, 1, :]
                        psr_ = psf.next()
                        psi_ = psf.next()

                        def f(psr_=psr_, psi_=psi_, Dr=Dr, Di=Di):
                            nc_t.matmul(psr_.h[:], lhsT=f128.h[:, 0, :], rhs=Dr, start=True, stop=False)
                            nc_t.matmul(psr_.h[:], lhsT=f128.h[:, 1, :], rhs=Di, start=False, stop=True)
                            nc_t.matmul(psi_.h[:], lhsT=f128.h[:, 0, :], rhs=Di, start=True, stop=False)
                            return nc_t.matmul(psi_.h[:], lhsT=f128.h[:, 2, :], rhs=Dr, start=False, stop=True)

                        kb.op(PE, f, r=[d0.d, f128.d], w=[psr_.d, psi_.d])
                        t1 = t1r.next()
                        t2 = t2r.next()
                        d1 = d1r.next()
                        kb.op(ACT, lambda t1=t1, psi_=psi_, l2=l2: nc_s.activation(
                            out=t1.h[:], in_=psi_.h[:], func=AF.Identity, scale=tw.h[:, 1, l2:l2 + 1]),
                              r=[psi_.d, tw.d], w=[t1.d])
                        kb.op(ACT, lambda t2=t2, psi_=psi_, l2=l2: nc_s.activation(
                            out=t2.h[:], in_=psi_.h[:], func=AF.Identity, scale=tw.h[:, 0, l2:l2 + 1]),
                              r=[psi_.d, tw.d], w=[t2.d])
                        kb.op(DVE, lambda d1=d1, psr_=psr_, t1=t1, l2=l2: nc_v.scalar_tensor_tensor(
                            out=d1.h[:, 0:512], in0=psr_.h[:], scalar=tw.h[:, 0, l2:l2 + 1], in1=t1.h[:],
                            op0=ALU.mult, op1=ALU.subtract), r=[psr_.d, t1.d, tw.d], w=[d1.d])
                        kb.op(DVE, lambda d1=d1, psr_=psr_, t2=t2, l2=l2: nc_v.scalar_tensor_tensor(
                            out=d1.h[:, 512:1024], in0=psr_.h[:], scalar=tw.h[:, 1, l2:l2 + 1], in1=t2.h[:],
                            op0=ALU.mult, op1=ALU.add), r=[psr_.d, t2.d, tw.d], w=[d1.d])
                        kb.dma(SP, D1[:, l2, :], d1.h[:], r=[d1.d])
                    kb.barrier()
                    if stop_after == "B2":
                        return nc, phases_done
                with contextlib.ExitStack() as sub:
                    f64 = kb.sb(sub, [64, 2, NT], BF16, "f64")
                    kb.dma(POOL, f64.h[:], c_f64.rearrange("a p n -> p a n"), w=[f64.d])
                    dr = kb.ring(sub, 3, [64, 4, D], BF16, "d3")
                    yr = kb.ring(sub, 3, [NT, 4, 512], BF16, "y3")
                    def ld_b3(kbk):
                        dd = dr.next()
                        kb.dma(SP, dd.h[:], D1[kbk * 4:(kbk + 1) * 4, :, :].rearrange("k l c -> l k c"), w=[dd.d])
                        return dd

                    pfx = Prefetch(range(32), ld_b3, 2)
                    for kbk in range(32):
                        dd = pfx.get(kbk)
                        yt = yr.next()
                        for kk in range(4):
                            ps = psf.next()

                            def f(ps=ps, dd=dd, kk=kk):
                                nc_t.matmul(ps.h[0:NT, :], lhsT=f64.h[:, 0, :], rhs=dd.h[:, kk, 0:512],
                                            start=True, stop=False)
                                return nc_t.matmul(ps.h[0:NT, :], lhsT=f64.h[:, 1, :], rhs=dd.h[:, kk, 512:1024],
                                                   start=False, stop=True)

                            kb.op(PE, f, r=[dd.d, f64.d], w=[ps.d])
                            if kk % 2 == 0:
                                kb.op(ACT, lambda ps=ps, yt=yt, kk=kk: nc_s.copy(out=yt.h[:, kk, :], in_=ps.h[0:NT, :]),
                                      r=[ps.d], w=[yt.d])
                            else:
                                kb.op(DVE, lambda ps=ps, yt=yt, kk=kk: nc_v.tensor_copy(out=yt.h[:, kk, :],
                                                                                        in_=ps.h[0:NT, :]),
                                      r=[ps.d], w=[yt.d])
                        kb.dma(SP, Yf[:, kbk * 4:(kbk + 1) * 4, :], yt.h[:], r=[yt.d])
            if done("B"):
                return nc, phases_done

            with contextlib.ExitStack() as cd:
                KT = kb.sb(cd, [64, NTK, 2, 128], BF16, "KT")
                Vaug = kb.sb(cd, [128, NTK, 2, 65], BF16, "Vaug")
                KTd = [Dep() for _ in range(NTK)]
                Vd = [Dep() for _ in range(NTK)]
                with contextlib.ExitStack() as ph:
                    ncx = NormCtx(ph)
                    mod = prep_mod(ph, 0, 0, ["G1", "S1"], ncx)
                    modc = prep_mod(ph, 0, 1, ["G1", "S1"], ncx)
                    Wqkv = kb.sb(ph, [128, 8, 768], BF16, "Wqkv")
                    load_w_bf(Wqkv, w_in[:, 512:1280])
                    psf = kb.ring(ph, 5, [128, 512], F32, "psC", psum=True)
                    psb = kb.ring(ph, 3, [128, 1024], BF16, "psCb", psum=True)
                    xr = kb.ring(ph, 3, [128, D], F32, "xC")
                    hr = kb.ring(ph, 3, [128, D], BF16, "hC")
                    hTr = kb.ring(ph, 3, [128, 8, 128], BF16, "hTC")
                    cosr = kb.ring(ph, 3, [128, 256], F32, "cosC")
                    sinr = kb.ring(ph, 3, [128, 256], F32, "sinC")
                    ta = kb.ring(ph, 4, [128, 256], F32, "ropA")
                    tb = kb.ring(ph, 4, [128, 256], F32, "ropB")
                    qrr = kb.ring(ph, 3, [128, 512], BF16, "qr")
                    krr = kb.ring(ph, 3, [128, 128], BF16, "kr")
                    qtr = kb.ring(ph, 3, [64, 1024], BF16, "qtC")
                    def ld_c(i):
                        xt = xr.next()
                        src = xw[i * 128:(i + 1) * 128, :] if i < NT else ctxb[(i - NT) * 128:(i - NT + 1) * 128, :]
                        kb.dma(SP, xt.h[:], src, w=[xt.d])
                        cs = cosr.next()
                        sn = sinr.next()
                        kb.dma(SP, cs.h[:], c_cos[i * 128:(i + 1) * 128, :], w=[cs.d])
                        kb.dma(SP, sn.h[:], c_sin[i * 128:(i + 1) * 128, :], w=[sn.d])
                        return xt, cs, sn

                    pfx = Prefetch(range(NTK), ld_c, 2)
                    for i in range(NTK):
                        xt, cs, sn = pfx.get(i)
                        m = mod if i < NT else modc
                        h = hr.next()
                        norm_mod(ncx, xt.h[:], [xt.d], m["G1"], m["S1"], h.h[:], [h.d])
                        hT = hTr.next()
                        transposes_bf(psb, [h.h[:, k * 128:(k + 1) * 128] for k in range(8)], [h.d],
                                      hT.h[:].rearrange("p k t -> p (k t)"), [hT.d], ACT)
                        psq = psf.next()
                        pskv = psf.next()
                        linear(psq.h[:], psq.d, hT, Wqkv, 0, 512)
                        linear(pskv.h[:, 0:256], pskv.d, hT, Wqkv, 512, 256)
                        qr = qrr.next()
                        kr = krr.next()
                        for (src_ap, srcd, dstt, na) in ((psq.h[:, 0:512], psq.d, qr, 16), (pskv.h[:, 0:128], pskv.d, kr, 4)):
                            sv = src_ap.rearrange("p (a two f) -> p a two f", two=2, f=16)
                            x1 = sv[:, :, 0, :]
                            x2 = sv[:, :, 1, :]
                            dv = dstt.h[:].rearrange("p (a two f) -> p a two f", two=2, f=16)
                            cv = cs.h[:, 0:na * 16].rearrange("p (a f) -> p a f", f=16)
                            svn = sn.h[:, 0:na * 16].rearrange("p (a f) -> p a f", f=16)
                            A = ta.next()
                            B = tb.next()
                            Av = A.h[:, 0:na * 16].rearrange("p (a f) -> p a f", f=16)
                            Bv = B.h[:, 0:na * 16].rearrange("p (a f) -> p a f", f=16)
                            kb.op(DVE, lambda Av=Av, x1=x1, cv=cv: nc_v.tensor_tensor(out=Av, in0=x1, in1=cv, op=ALU.mult),
                                  r=[srcd, cs.d], w=[A.d])
                            kb.op(DVE, lambda Bv=Bv, x2=x2, svn=svn: nc_v.tensor_tensor(out=Bv, in0=x2, in1=svn, op=ALU.mult),
                                  r=[srcd, sn.d], w=[B.d])
                            kb.op(POOL, lambda dv=dv, Av=Av, Bv=Bv: nc_g.tensor_tensor(out=dv[:, :, 0, :], in0=Av, in1=Bv,
                                                                                       op=ALU.subtract),
                                  r=[A.d, B.d], w=[dstt.d])
                            A2 = ta.next()
                            B2 = tb.next()
                            A2v = A2.h[:, 0:na * 16].rearrange("p (a f) -> p a f", f=16)
                            B2v = B2.h[:, 0:na * 16].rearrange("p (a f) -> p a f", f=16)
                            kb.op(DVE, lambda A2v=A2v, x1=x1, svn=svn: nc_v.tensor_tensor(out=A2v, in0=x1, in1=svn, op=ALU.mult),
                                  r=[srcd, sn.d], w=[A2.d])
                            kb.op(DVE, lambda B2v=B2v, x2=x2, cv=cv: nc_v.tensor_tensor(out=B2v, in0=x2, in1=cv, op=ALU.mult),
                                  r=[srcd, cs.d], w=[B2.d])
                            kb.op(POOL, lambda dv=dv, A2v=A2v, B2v=B2v: nc_g.tensor_tensor(out=dv[:, :, 1, :], in0=A2v, in1=B2v,
                                                                                          op=ALU.add),
                                  r=[A2.d, B2.d], w=[dstt.d])
                        kb.op(DVE, lambda i=i, pskv=pskv: nc_v.tensor_scalar(
                            out=Vaug.h[:, i, :, 0:64], in0=pskv.h[:, 128:256].rearrange("p (a d) -> p a d", a=2),
                            scalar1=tvalid.h[:, i:i + 1], scalar2=None, op0=ALU.mult),
                              r=[pskv.d, tvalid.d], w=[Vd[i]])
                        kb.op(POOL, lambda i=i: nc_g.tensor_scalar(
                            out=Vaug.h[:, i, :, 64:65], in0=ones2.h[:], scalar1=tvalid.h[:, i:i + 1], scalar2=None,
                            op0=ALU.mult), r=[ones2.d, tvalid.d], w=[Vd[i]])
                        qt = qtr.next()
                        transposes_bf(psb, [qr.h[:, hh * 64:(hh + 1) * 64] for hh in range(8)], [qr.d],
                                      qt.h[:], [qt.d], ACT, rows=64)
                        transposes_bf(psb, [kr.h[:, hh * 64:(hh + 1) * 64] for hh in range(2)], [kr.d],
                                      KT.h[:, i, :, :].rearrange("p a t -> p (a t)"), [KTd[i]], DVE, rows=64)
                        if i < NT:
                            kb.dma(SP, QTs[i], qt.h[:], r=[qt.d])
                if done("C"):
                    return nc, phases_done

                with contextlib.ExitStack() as ph:
                    post = PostMix(kb, nc, ph, identf, epst, depth=3)
                    Wout = kb.sb(ph, [128, 8, D], BF16, "Wout")
                    load_w_bf(Wout, w_out)
                    masks = kb.sb(ph, [128, 2, 512], BF16, "masks")
                    kb.dma(POOL, masks.h[:], c_mask.rearrange("a p n -> p a n"), w=[masks.d])
                    esink = kb.sb(ph, [128, 8], F32, "esink")
                    kb.dma(SP, esink.h[:], sink.partition_broadcast(128), w=[esink.d])
                    kb.op(ACT, lambda: nc_s.activation(out=esink.h[:], in_=esink.h[:], func=AF.Exp), w=[esink.d])
                    psf = kb.ring(ph, 6, [128, 512], F32, "psD", psum=True)
                    psb = kb.ring(ph, 2, [128, 1024], BF16, "psDb", psum=True)
                    post.setup(router_w, router_b, psf)
                    ncx = NormCtx(ph)
                    mod = prep_mod(ph, 0, 0, ["g1", "G2", "S2"], ncx)
                    xr = kb.ring(ph, 3, [128, D], F32, "xD")
                    qtr = kb.ring(ph, 3, [64, 1024], BF16, "qtD")
                    mcr = kb.ring(ph, 3, [128, D], BF16, "mcD")
                    mcTr = kb.ring(ph, 3, [128, 8, 128], BF16, "mcT")
                    ptr = kb.ring(ph, 15, [128, 512], BF16, "pt")
                    denr = kb.ring(ph, 4, [128, 8], F32, "den")
                    zf = xr.next()
                    zb = mcr.next()
                    kb.op(POOL, lambda: nc_g.memset(zf.h[:], 0.0), w=[zf.d])
                    kb.op(POOL, lambda: nc_g.memset(zb.h[:], 0.0), w=[zb.d])
                    for zi in (0, NT - 1):
                        kb.dma(SP, X1[zi * 128:(zi + 1) * 128, :], zf.h[:], r=[zf.d])
                        kb.dma(SP, FTs[zi], zb.h[:], r=[zb.d])
                    def ld_d(i):
                        xt = xr.next()
                        kb.dma(SP, xt.h[:], xw[i * 128:(i + 1) * 128, :], w=[xt.d])
                        qt = qtr.next()
                        kb.dma(SP, qt.h[:], QTs[i], w=[qt.d])
                        mc = mcr.next()
                        kb.dma(SP, mc.h[:, 0:512], Yf[i], w=[mc.d])
                        return xt, qt, mc

                    pfx = Prefetch(L0, ld_d, 2)
                    for ki, i in enumerate(L0):
                        xt, qt, mc = pfx.get(ki)
                        for kv in range(2):
                            keys = [36, 37, i - 1, i, i + 1]
                            pts = []
                            for jj, j in enumerate(keys):
                                pss = psf.next()
                                kb.op(PE, lambda pss=pss, j=j, kv=kv, qt=qt: nc_t.matmul(
                                    pss.h[:], lhsT=KT.h[:, j, kv, :], rhs=qt.h[:, kv * 512:(kv + 1) * 512],
                                    start=True, stop=True), r=[KTd[j], qt.d], w=[pss.d])
                                pt = ptr.next()
                                kb.op(ACT, lambda pss=pss, pt=pt: nc_s.activation(out=pt.h[:], in_=pss.h[:], func=AF.Exp,
                                                                                   scale=0.125), r=[pss.d], w=[pt.d])
                                if jj in (2, 4):
                                    mi = 0 if jj == 2 else 1
                                    kb.op(POOL, lambda pt=pt, mi=mi: nc_g.tensor_tensor(out=pt.h[:], in0=pt.h[:],
                                                                                        in1=masks.h[:, mi, :], op=ALU.mult),
                                          r=[masks.d], w=[pt.d])
                                pts.append(pt)
                            pso = psf.next()

                            def f(pso=pso, pts=pts, keys=keys, kv=kv):
                                ins = None
                                for g in range(4):
                                    for jj, j in enumerate(keys):
                                        ins = nc_t.matmul(pso.h[:, g * 65:(g + 1) * 65],
                                                          lhsT=pts[jj].h[:, g * 128:(g + 1) * 128],
                                                          rhs=Vaug.h[:, j, kv, :], start=(jj == 0), stop=(jj == 4))
                                return ins

                            kb.op(PE, f, r=[p.d for p in pts] + [Vd[j] for j in keys], w=[pso.d])
                            den = denr.next()
                            pov = pso.h[:, 0:260].rearrange("p (g e) -> p g e", e=65)
                            kb.op(DVE, lambda den=den, pov=pov, kv=kv: nc_v.tensor_tensor(
                                out=den.h[:, 0:4], in0=pov[:, :, 64], in1=esink.h[:, kv * 4:(kv + 1) * 4], op=ALU.add),
                                  r=[pso.d, esink.d], w=[den.d])
                            kb.op(DVE, lambda den=den: nc_v.reciprocal(out=den.h[:, 4:8], in_=den.h[:, 0:4]), w=[den.d])
                            for g in range(4):
                                hh = kv * 4 + g
                                kb.op(DVE, lambda mc=mc, pso=pso, den=den, g=g, hh=hh: nc_v.tensor_scalar(
                                    out=mc.h[:, 512 + hh * 64:512 + (hh + 1) * 64], in0=pso.h[:, g * 65:g * 65 + 64],
                                    scalar1=den.h[:, 4 + g:5 + g], scalar2=None, op0=ALU.mult),
                                      r=[pso.d, den.d], w=[mc.d])
                        mcT = mcTr.next()
                        transposes_bf(psb, [mc.h[:, k * 128:(k + 1) * 128] for k in range(8)], [mc.d],
                                      mcT.h[:].rearrange("p k t -> p (k t)"), [mcT.d], ACT)
                        pm = [psf.next(), psf.next()]
                        for c in range(2):
                            linear(pm[c].h[:], pm[c].d, mcT, Wout, c * 512, 512)
                        post.run(i, pm, None, xt, mod, ncx, X1, FTs, wts[0])
                    post.routing(wts[0])
                if done("D"):
                    return nc, phases_done

            moe_phase(kb, nc, 0, L0, MOD[0, 0:1, 5 * D:6 * D], X1, X2, FTs, wts[0], moe_g, moe_u, moe_d, None, None, epst)
            if done("E"):
                return nc, phases_done

        with contextlib.ExitStack() as lay:
            convsc = contextlib.ExitStack()
            DG = kb.sb(convsc, [128, 8, 31, 128], BF16, "DG")
            dwT = kb.sb(convsc, [128, 8, 31], F32, "dwT")
            kb.dma(SP, dwT.h[:], dw_wT.rearrange("(c p) j -> p c j", p=128), w=[dwT.d])
            DGd = [Dep() for _ in range(8)]
            for c in range(8):
                for j in range(31):
                    kb.op(ACT, lambda c=c, j=j: nc_s.activation(out=DG.h[:, c, j, :], in_=identb.h[:], func=AF.Identity,
                                                                scale=dwT.h[:, c, j:j + 1]),
                          r=[identb.d, dwT.d], w=[DGd[c]])
            with contextlib.ExitStack() as ph:
                Wpw1 = kb.sb(ph, [128, 8, 2 * D], BF16, "Wpw1")
                load_w_bf(Wpw1, pw1_w)
                b1 = kb.sb(ph, [128, 2 * D], F32, "b1")
                bc_load(b1, pw1_b)
                psf = kb.ring(ph, 6, [128, 512], F32, "psF", psum=True)
                psb = kb.ring(ph, 2, [128, 1024], BF16, "psFb", psum=True)
                ncx = NormCtx(ph)
                mod = prep_mod(ph, 1, 0, ["G1", "S1"], ncx)
                xr = kb.ring(ph, 3, [128, D], F32, "xF")
                hr = kb.ring(ph, 3, [128, D], BF16, "hF")
                hTr = kb.ring(ph, 3, [128, 8, 128], BF16, "hTF")
                tgr = kb.ring(ph, 3, [128, 512], F32, "tg")
                sgr = kb.ring(ph, 3, [128, 512], F32, "sg")
                tar = kb.ring(ph, 3, [128, 512], F32, "taF")
                tur = kb.ring(ph, 3, [128, 512], F32, "tuF")
                ur = kb.ring(ph, 3, [128, D], BF16, "uF")
                uTr = kb.ring(ph, 3, [128, 8, 128], BF16, "uTF")
                def ld_f(i):
                    xt = xr.next()
                    kb.dma(SP, xt.h[:], X2[i * 128:(i + 1) * 128, :], w=[xt.d])
                    return xt

                pfx = Prefetch(L0, ld_f, 2)
                for ki, i in enumerate(L0):
                    xt = pfx.get(ki)
                    h = hr.next()
                    norm_mod(ncx, xt.h[:], [xt.d], mod["G1"], mod["S1"], h.h[:], [h.d])
                    hT = hTr.next()
                    transposes_bf(psb, [h.h[:, k * 128:(k + 1) * 128] for k in range(8)], [h.d],
                                  hT.h[:].rearrange("p k t -> p (k t)"), [hT.d], ACT)
                    pp = [psf.next() for _ in range(4)]
                    for c in range(4):
                        linear(pp[c].h[:], pp[c].d, hT, Wpw1, c * 512, 512)
                    u = ur.next()
                    for hf in range(2):
                        tg = tgr.next()
                        sg = sgr.next()
                        tA = tar.next()
                        tu = tur.next()
                        kb.op(DVE, lambda tg=tg, hf=hf, pp=pp: nc_v.tensor_tensor(
                            out=tg.h[:], in0=pp[2 + hf].h[:], in1=b1.h[:, D + hf * 512:D + (hf + 1) * 512], op=ALU.add),
                              r=[pp[2 + hf].d, b1.d], w=[tg.d])
                        kb.op(ACT, lambda tg=tg, sg=sg: nc_s.activation(out=sg.h[:], in_=tg.h[:], func=AF.Sigmoid),
                              r=[tg.d], w=[sg.d])
                        kb.op(DVE, lambda tA=tA, hf=hf, pp=pp: nc_v.tensor_tensor(
                            out=tA.h[:], in0=pp[hf].h[:], in1=b1.h[:, hf * 512:(hf + 1) * 512], op=ALU.add),
                              r=[pp[hf].d, b1.d], w=[tA.d])
                        if i in OWN:
                            kb.op(POOL, lambda u=u, tA=tA, sg=sg, hf=hf: nc_g.tensor_tensor(
                                out=u.h[:, hf * 512:(hf + 1) * 512], in0=tA.h[:], in1=sg.h[:], op=ALU.mult),
                                  r=[tA.d, sg.d], w=[u.d])
                        else:
                            kb.op(POOL, lambda tu=tu, tA=tA, sg=sg: nc_g.tensor_tensor(out=tu.h[:], in0=tA.h[:], in1=sg.h[:],
                                                                                       op=ALU.mult), r=[tA.d, sg.d], w=[tu.d])
                            kb.op(POOL, lambda u=u, tu=tu, hf=hf, i=i: nc_g.tensor_scalar(
                                out=u.h[:, hf * 512:(hf + 1) * 512], in0=tu.h[:], scalar1=tvalid.h[:, i:i + 1], scalar2=None,
                                op0=ALU.mult), r=[tu.d, tvalid.d], w=[u.d])
                    uT = uTr.next()
                    transposes_bf(psb, [u.h[:, k * 128:(k + 1) * 128] for k in range(8)], [u.d],
                                  uT.h[:].rearrange("p k t -> p (k t)"), [uT.d], ACT)
                    kb.dma(SP, UTs[:, :, (i - 1) * 128:i * 128], uT.h[:], r=[uT.d])
            if done("F"):
                return nc, phases_done

            with contextlib.ExitStack() as ph:
                post = PostMix(kb, nc, ph, identf, epst)
                Wpw2 = kb.sb(ph, [128, 8, D], BF16, "Wpw2")
                load_w_bf(Wpw2, pw2_w)
                dwb = kb.sb(ph, [128, D], F32, "dwb")
                lng = kb.sb(ph, [128, D], F32, "lng")
                lnb = kb.sb(ph, [128, D], F32, "lnb")
                b2 = kb.sb(ph, [128, D], F32, "b2")
                bc_load(dwb, dw_b)
                bc_load(lng, ln_g)
                bc_load(lnb, ln_b)
                bc_load(b2, pw2_b)
                psf = kb.ring(ph, 6, [128, 512], F32, "psG", psum=True)
                psb = kb.ring(ph, 2, [128, 1024], BF16, "psGb", psum=True)
                post.setup(router_w, router_b, psf)
                ncx = NormCtx(ph, 2)
                mod = prep_mod(ph, 1, 0, ["g1", "G2", "S2"], ncx)
                xr = kb.ring(ph, 2, [128, D], F32, "xG")
                uwr = kb.ring(ph, 2, [128, 8, 158], BF16, "uw")
                vr = kb.ring(ph, 1, [128, D], F32, "vG")
                bnr = kb.ring(ph, 2, [128, 16], F32, "bnG")
                nr = kb.ring(ph, 1, [128, D], F32, "nG")
                sr = kb.ring(ph, 2, [128, D], BF16, "sG")
                sTr = kb.ring(ph, 2, [128, 8, 128], BF16, "sTG")
                def ld_g(i):
                    xt = xr.next()
                    kb.dma(SP, xt.h[:], X2[i * 128:(i + 1) * 128, :], w=[xt.d])
                    uw = uwr.next()
                    t0 = (i - 1) * 128 - 15
                    kb.dma(SP, uw.h[:], UTs[:, :, t0:t0 + 158], w=[uw.d])
                    return xt, uw

                pfx = Prefetch(OWN, ld_g, 1)
                for ki, i in enumerate(OWN):
                    xt, uw = pfx.get(ki)
                    pc = [psf.next(), psf.next()]

                    def f(pc=pc, uw=uw):
                        ins = None
                        for c in range(8):
                            for j in range(31):
                                ins = nc_t.matmul(pc[c // 4].h[:, (c % 4) * 128:(c % 4 + 1) * 128],
                                                  lhsT=uw.h[:, c, j:j + 128], rhs=DG.h[:, c, j, :],
                                                  start=(j == 0), stop=(j == 30))
                        return ins

                    kb.op(PE, f, r=[uw.d] + DGd, w=[pc[0].d, pc[1].d])
                    v = vr.next()
                    bn = bnr.next()
                    for hf in range(2):
                        kb.op(DVE, lambda v=v, pc=pc, hf=hf: nc_v.tensor_tensor(
                            out=v.h[:, hf * 512:(hf + 1) * 512], in0=pc[hf].h[:], in1=dwb.h[:, hf * 512:(hf + 1) * 512],
                            op=ALU.add), r=[pc[hf].d, dwb.d], w=[v.d])
                    for hf in range(2):
                        kb.op(DVE, lambda v=v, bn=bn, hf=hf: nc_v.bn_stats(out=bn.h[:, hf * 6:(hf + 1) * 6],
                                                                           in_=v.h[:, hf * 512:(hf + 1) * 512]),
                              r=[v.d], w=[bn.d])
                    kb.op(DVE, lambda bn=bn: nc_v.bn_aggr(out=bn.h[:, 12:14], in_=bn.h[:, 0:12]), w=[bn.d])
                    kb.op(ACT, lambda bn=bn: nc_s.activation(out=bn.h[:, 14:15], in_=bn.h[:, 13:14], func=AF.Sqrt,
                                                             bias=epst.h[:, 0:1], scale=1.0), r=[epst.d], w=[bn.d])
                    kb.op(DVE, lambda bn=bn: nc_v.reciprocal(out=bn.h[:, 15:16], in_=bn.h[:, 14:15]), w=[bn.d])
                    n_ = nr.next()
                    kb.op(DVE, lambda n_=n_, v=v, bn=bn: nc_v.tensor_scalar(
                        out=n_.h[:], in0=v.h[:], scalar1=bn.h[:, 12:13], scalar2=bn.h[:, 15:16], op0=ALU.subtract,
                        op1=ALU.mult), r=[v.d, bn.d], w=[n_.d])
                    kb.op(POOL, lambda n_=n_: nc_g.tensor_tensor(out=n_.h[:], in0=n_.h[:], in1=lng.h[:], op=ALU.mult),
                          r=[lng.d], w=[n_.d])
                    kb.op(POOL, lambda n_=n_: nc_g.tensor_tensor(out=n_.h[:], in0=n_.h[:], in1=lnb.h[:], op=ALU.add),
                          r=[lnb.d], w=[n_.d])
                    s_ = sr.next()
                    kb.op(ACT, lambda s_=s_, n_=n_: nc_s.activation(out=s_.h[:], in_=n_.h[:], func=AF.Silu),
                          r=[n_.d], w=[s_.d])
                    sT = sTr.next()
                    transposes_bf(psb, [s_.h[:, k * 128:(k + 1) * 128] for k in range(8)], [s_.d],
                                  sT.h[:].rearrange("p k t -> p (k t)"), [sT.d], ACT)
                    pm = [psf.next(), psf.next()]
                    for c in range(2):
                        linear(pm[c].h[:], pm[c].d, sT, Wpw2, c * 512, 512)
                    post.run(i, pm, b2, xt, mod, ncx, X3, FTs, wts[1])
                post.routing(wts[1])
            if done("G"):
                return nc, phases_done
            convsc.close()

            moe_phase(kb, nc, 1, OWN, MOD[1, 0:1, 5 * D:6 * D], X3, None, FTs, wts[1], moe_g, moe_u, moe_d, out, fin_g, epst)
        kb.barrier()
        phases_done.append("H")
    return nc, phases_done


class PostMix:
    def __init__(self, kb, nc, scope, identf, epst, depth=2):
        self.kb, self.nc, self.scope, self.identf, self.epst = kb, nc, scope, identf, epst
        self.depth = depth

    def setup(self, router_w, router_b, psf):
        kb, nc, ph = self.kb, self.nc, self.scope
        self.psf = psf
        self.rw = kb.sb(ph, [128, 8, NE], F32, "rw")
        kb.dma(kb.SP, self.rw.h[:], router_w.rearrange("(k p) n -> p k n", p=128), w=[self.rw.d])
        self.rb = kb.sb(ph, [128, NE], F32, "rb")
        kb.dma(kb.SP, self.rb.h[:], router_b.partition_broadcast(128), w=[self.rb.d])
        self.sc_all = kb.sb(ph, [128, NT, NE], F32, "sc_all")
        kb.op(kb.POOL, lambda: nc.gpsimd.memset(self.sc_all.h[:], 0.5), w=[self.sc_all.d])
        self.tmpr = kb.ring(ph, self.depth, [128, D], F32, "pmtmp")
        self.x1r = kb.ring(ph, self.depth, [128, D], F32, "pmx1")
        self.fr = kb.ring(ph, self.depth, [128, D], F32, "pmf")
        self.fTr = kb.ring(ph, self.depth, [128, 8, 128], F32, "pmfT")
        self.fbr = kb.ring(ph, self.depth, [128, 8, 128], BF16, "pmfb")

    def run(self, i, pm, bias, xt, mod, ncx, Xout, FTs, wts):
        kb, nc = self.kb, self.nc
        nc_v, nc_s, nc_g, nc_t = nc.vector, nc.scalar, nc.gpsimd, nc.tensor
        DVE, POOL, ACT, PE, SP = kb.DVE, kb.POOL, kb.ACT, kb.PE, kb.SP
        tmp = self.tmpr.next()
        x1 = self.x1r.next()
        for c in range(2):
            sl = slice(c * 512, (c + 1) * 512)
            if bias is not None:
                kb.op(DVE, lambda c=c, sl=sl: nc_v.tensor_tensor(out=tmp.h[:, sl], in0=pm[c].h[:], in1=bias.h[:, sl],
                                                                 op=ALU.add), r=[pm[c].d, bias.d], w=[tmp.d])
                kb.op(POOL, lambda sl=sl: nc_g.tensor_tensor(out=tmp.h[:, sl], in0=tmp.h[:, sl], in1=mod["g1"].h[:, sl],
                                                             op=ALU.mult), r=[mod["g1"].d], w=[tmp.d])
            else:
                kb.op(DVE, lambda c=c, sl=sl: nc_v.tensor_tensor(out=tmp.h[:, sl], in0=pm[c].h[:],
                                                                 in1=mod["g1"].h[:, sl], op=ALU.mult),
                      r=[pm[c].d, mod["g1"].d], w=[tmp.d])
        kb.op(POOL, lambda: nc_g.tensor_tensor(out=x1.h[:], in0=tmp.h[:], in1=xt.h[:], op=ALU.add),
              r=[tmp.d, xt.d], w=[x1.d])
        kb.dma(SP, Xout[i * 128:(i + 1) * 128, :], x1.h[:], r=[x1.d])
        f = self.fr.next()
        junk = ncx.junk.next()
        st = ncx.stat.next()
        kb.op(ACT, lambda: nc_s.activation(out=junk.h[:], in_=x1.h[:], func=AF.Square, scale=1.0 / 32.0,
                                           accum_out=st.h[:, 0:1]), r=[x1.d], w=[junk.d, st.d])
        kb.op(ACT, lambda: nc_s.activation(out=st.h[:, 1:2], in_=st.h[:, 0:1], func=AF.Sqrt,
                                           bias=self.epst.h[:, 0:1], scale=1.0), r=[self.epst.d], w=[st.d])
        kb.op(DVE, lambda: nc_v.reciprocal(out=st.h[:, 2:3], in_=st.h[:, 1:2]), w=[st.d])
        t2 = ncx.tmp.next()
        kb.op(DVE, lambda: nc_v.scalar_tensor_tensor(out=t2.h[:], in0=x1.h[:], scalar=st.h[:, 2:3], in1=mod["G2"].h[:],
                                                     op0=ALU.mult, op1=ALU.mult), r=[x1.d, st.d, mod["G2"].d], w=[t2.d])
        kb.op(POOL, lambda: nc_g.tensor_tensor(out=f.h[:], in0=t2.h[:], in1=mod["S2"].h[:], op=ALU.add),
              r=[t2.d, mod["S2"].d], w=[f.d])
        fT = self.fTr.next()
        for hf in range(2):
            ps = self.psf.next()

            def g(ps=ps, hf=hf):
                ins = None
                for k in range(4):
                    kk = hf * 4 + k
                    ins = nc_t.transpose(out=ps.h[:, k * 128:(k + 1) * 128], in_=f.h[:, kk * 128:(kk + 1) * 128],
                                         identity=self.identf.h[:])
                return ins

            kb.op(PE, g, r=[f.d, self.identf.d], w=[ps.d])
            kb.op(ACT, lambda ps=ps, hf=hf: nc_s.copy(out=fT.h[:, hf * 4:(hf + 1) * 4, :].rearrange("p k t -> p (k t)"),
                                                      in_=ps.h[:]), r=[ps.d], w=[fT.d])
        fb = self.fbr.next()
        kb.op(POOL, lambda: nc_g.tensor_copy(out=fb.h[:], in_=fT.h[:]), r=[fT.d], w=[fb.d])
        kb.dma(SP, FTs[i], fb.h[:].rearrange("p k t -> p (k t)"), r=[fb.d])
        psl = self.psf.next()

        def g2():
            ins = None
            for k in range(8):
                ins = nc_t.matmul(psl.h[:, 0:NE], lhsT=fT.h[:, k, :], rhs=self.rw.h[:, k, :], start=(k == 0), stop=(k == 7))
            return ins

        kb.op(PE, g2, r=[fT.d, self.rw.d], w=[psl.d])
        kb.op(ACT, lambda: nc_s.activation(out=self.sc_all.h[:, i, :], in_=psl.h[:, 0:NE], func=AF.Sigmoid),
              r=[psl.d], w=[self.sc_all.d])

    def routing(self, wts):
        kb, nc, ph = self.kb, self.nc, self.scope
        nc_v = nc.vector
        DVE = kb.DVE
        T = NT
        sc = self.sc_all
        bi = kb.sb(ph, [128, T, NE], F32, "r_bi")
        b2 = kb.sb(ph, [128, T, NE], F32, "r_b2")
        e1 = kb.sb(ph, [128, T, NE], F32, "r_e1")
        e2 = kb.sb(ph, [128, T, NE], F32, "r_e2")
        m1 = kb.sb(ph, [128, T, 4], F32, "r_m1")
        m2 = kb.sb(ph, [128, T, 4], F32, "r_m2")
        gs = kb.sb(ph, [128, T, 4], F32, "r_gs")
        gm = kb.sb(ph, [128, T], F32, "r_gm")
        ws = kb.sb(ph, [128, T], F32, "r_ws")
        v4 = lambda t: t.h[:].rearrange("p t (g e) -> p t g e", g=4)
        bc4 = lambda t: t.h[:].unsqueeze(3).broadcast_to([128, T, 4, 4])
        kb.op(DVE, lambda: nc_v.tensor_tensor(out=bi.h[:], in0=sc.h[:], in1=self.rb.h[:].unsqueeze(1).broadcast_to([128, T, NE]),
                                              op=ALU.add), r=[sc.d, self.rb.d], w=[bi.d])
        kb.op(DVE, lambda: nc_v.tensor_reduce(out=m1.h[:], in_=v4(bi), axis=AX.X, op=ALU.max), r=[bi.d], w=[m1.d])
        kb.op(DVE, lambda: nc_v.tensor_tensor(out=v4(e1), in0=v4(bi), in1=bc4(m1), op=ALU.is_equal), r=[bi.d, m1.d], w=[e1.d])
        kb.op(DVE, lambda: nc_v.scalar_tensor_tensor(out=b2.h[:], in0=e1.h[:], scalar=-1e9, in1=bi.h[:], op0=ALU.mult,
                                                     op1=ALU.add), r=[e1.d, bi.d], w=[b2.d])
        kb.op(DVE, lambda: nc_v.tensor_reduce(out=m2.h[:], in_=v4(b2), axis=AX.X, op=ALU.max), r=[b2.d], w=[m2.d])
        kb.op(DVE, lambda: nc_v.tensor_tensor(out=gs.h[:], in0=m1.h[:], in1=m2.h[:], op=ALU.add), r=[m1.d, m2.d], w=[gs.d])
        kb.op(DVE, lambda: nc_v.tensor_reduce(out=gm.h[:], in_=gs.h[:], axis=AX.X, op=ALU.max), r=[gs.d], w=[gm.d])
        kb.op(DVE, lambda: nc_v.tensor_tensor(out=gs.h[:], in0=gs.h[:], in1=gm.h[:].unsqueeze(2).broadcast_to([128, T, 4]),
                                              op=ALU.is_equal), r=[gm.d], w=[gs.d])
        kb.op(DVE, lambda: nc_v.tensor_tensor(out=v4(e2), in0=v4(b2), in1=bc4(m2), op=ALU.is_equal), r=[b2.d, m2.d], w=[e2.d])
        kb.op(DVE, lambda: nc_v.tensor_tensor(out=e1.h[:], in0=e1.h[:], in1=e2.h[:], op=ALU.add), r=[e2.d], w=[e1.d])
        kb.op(DVE, lambda: nc_v.tensor_tensor(out=v4(e1), in0=v4(e1), in1=bc4(gs), op=ALU.mult), r=[gs.d], w=[e1.d])
        kb.op(DVE, lambda: nc_v.tensor_tensor(out=e1.h[:], in0=e1.h[:], in1=sc.h[:], op=ALU.mult), r=[sc.d], w=[e1.d])
        kb.op(DVE, lambda: nc_v.tensor_reduce(out=ws.h[:], in_=e1.h[:], axis=AX.X, op=ALU.add), r=[e1.d], w=[ws.d])
        kb.op(DVE, lambda: nc_v.reciprocal(out=ws.h[:], in_=ws.h[:]), w=[ws.d])
        kb.op(DVE, lambda: nc_v.tensor_tensor(out=wts.h[:], in0=e1.h[:], in1=ws.h[:].unsqueeze(2).broadcast_to([128, T, NE]),
                                              op=ALU.mult), r=[e1.d, ws.d], w=[wts.d])


def moe_phase(kb, nc, li, tiles, gate2_row, Xin, Xout, FTs, wts, moe_g, moe_u, moe_d, out, fin_g, epst):
    nc_v, nc_s, nc_g, nc_t = nc.vector, nc.scalar, nc.gpsimd, nc.tensor
    PE, ACT, DVE, POOL, SP = kb.PE, kb.ACT, kb.DVE, kb.POOL, kb.SP
    GMAX = 12
    groups = [tiles[a:a + GMAX] for a in range(0, len(tiles), GMAX)]
    with contextlib.ExitStack() as ph:
        fTgr = kb.ring(ph, 2, [128, GMAX, 8, 128], BF16, "fTg")
        yacc = kb.sb(ph, [128, GMAX, D], F32, "yacc")
        yd = [Dep() for _ in range(GMAX)]
        wgr = kb.ring(ph, 2, [128, 8, FF], BF16, "Wg")
        wur = kb.ring(ph, 2, [128, 8, FF], BF16, "Wu")
        wdr = kb.ring(ph, 2, [128, 4, D], BF16, "Wd")
        psgu = kb.ring(ph, 4, [128, 512], F32, "psgu", psum=True)
        psy = kb.ring(ph, 4, [128, 512], F32, "psy", psum=True)
        sgr = kb.ring(ph, 2, [128, 512], BF16, "sgt")
        hidr = kb.ring(ph, 2, [128, 4, 512], BF16, "hid")
        xr = kb.ring(ph, 2, [128, D], F32, "xE")
        tr = kb.ring(ph, 2, [128, D], F32, "tE")
        orr = kb.ring(ph, 2, [128, D], F32, "oE")
        gate2 = kb.sb(ph, [128, D], F32, "gate2")
        kb.dma(SP, gate2.h[:], gate2_row.partition_broadcast(128), w=[gate2.d])
        if out is not None:
            fg = kb.sb(ph, [128, D], F32, "fing")
            kb.dma(SP, fg.h[:], fin_g.partition_broadcast(128), w=[fg.d])
            junkr = kb.ring(ph, 2, [128, D], BF16, "junkE")
            str_ = kb.ring(ph, 2, [128, 4], F32, "stE")
        def ld_ft(grp):
            nt = len(grp)
            assert nt % 2 == 0 and grp == list(range(grp[0], grp[0] + nt))
            ft = fTgr.next()
            kb.dma(SP, ft.h[:, 0:nt, :, :].rearrange("p n k t -> p n (k t)"),
                   FTs[grp[0]:grp[0] + nt].rearrange("n p f -> p n f"), w=[ft.d])
            return ft

        pft = Prefetch(groups, ld_ft, 1)
        for gi, grp in enumerate(groups):
            nt = len(grp)
            fTg = pft.get(gi)
            for e in range(NE):
                Wg, Wu, Wd = wgr.next(), wur.next(), wdr.next()
                kb.dma(POOL, Wg.h[:], moe_g[li, e].rearrange("(k p) n -> p k n", p=128), w=[Wg.d])
                kb.dma(POOL, Wu.h[:], moe_u[li, e].rearrange("(k p) n -> p k n", p=128), w=[Wu.d])
                kb.dma(POOL, Wd.h[:], moe_d[li, e].rearrange("(k p) n -> p k n", p=128), w=[Wd.d])
                for sgi in range((nt + 3) // 4):
                    ntl = min(4, nt - sgi * 4)
                    NW = ntl * 128
                    hid = hidr.next()
                    for j in range(4):
                        pg = psgu.next()
                        pu = psgu.next()

                        def f(pg=pg, pu=pu, j=j, sgi=sgi, Wg=Wg, Wu=Wu, ntl=ntl, NW=NW, fTg=fTg):
                            ins = None
                            for k in range(8):
                                nc_t.matmul(pg.h[:, 0:NW], lhsT=Wg.h[:, k, j * 128:(j + 1) * 128],
                                            rhs=fTg.h[:, sgi * 4:sgi * 4 + ntl, k, :], start=(k == 0), stop=(k == 7))
                            for k in range(8):
                                ins = nc_t.matmul(pu.h[:, 0:NW], lhsT=Wu.h[:, k, j * 128:(j + 1) * 128],
                                                  rhs=fTg.h[:, sgi * 4:sgi * 4 + ntl, k, :], start=(k == 0), stop=(k == 7))
                            return ins

                        kb.op(PE, f, r=[Wg.d, Wu.d, fTg.d], w=[pg.d, pu.d])
                        sg = sgr.next()
                        kb.op(ACT, lambda sg=sg, pg=pg, NW=NW: nc_s.activation(out=sg.h[:, 0:NW], in_=pg.h[:, 0:NW], func=AF.Silu),
                              r=[pg.d], w=[sg.d])
                        kb.op(DVE, lambda hid=hid, j=j, sg=sg, pu=pu, NW=NW: nc_v.tensor_tensor(
                            out=hid.h[:, j, 0:NW], in0=pu.h[:, 0:NW], in1=sg.h[:, 0:NW], op=ALU.mult),
                              r=[pu.d, sg.d], w=[hid.d])
                    for t in range(ntl):
                        ti = sgi * 4 + t
                        tile_idx = grp[ti]
                        for c in range(2):
                            py = psy.next()

                            def f2(py=py, hid=hid, t=t, c=c, Wd=Wd):
                                ins = None
                                for j in range(4):
                                    ins = nc_t.matmul(py.h[:], lhsT=hid.h[:, j, t * 128:(t + 1) * 128],
                                                      rhs=Wd.h[:, j, c * 512:(c + 1) * 512], start=(j == 0), stop=(j == 3))
                                return ins

                            kb.op(PE, f2, r=[hid.d, Wd.d], w=[py.d])
                            if e == 0:
                                kb.op(DVE, lambda py=py, ti=ti, c=c, tile_idx=tile_idx, e=e: nc_v.tensor_scalar(
                                    out=yacc.h[:, ti, c * 512:(c + 1) * 512], in0=py.h[:],
                                    scalar1=wts.h[:, tile_idx, e:e + 1], scalar2=None, op0=ALU.mult),
                                      r=[py.d, wts.d], w=[yd[ti]])
                            else:
                                kb.op(DVE, lambda py=py, ti=ti, c=c, tile_idx=tile_idx, e=e: nc_v.scalar_tensor_tensor(
                                    out=yacc.h[:, ti, c * 512:(c + 1) * 512], in0=py.h[:], scalar=wts.h[:, tile_idx, e:e + 1],
                                    in1=yacc.h[:, ti, c * 512:(c + 1) * 512], op0=ALU.mult, op1=ALU.add),
                                      r=[py.d, wts.d], w=[yd[ti]])
            for ti, tile_idx in enumerate(grp):
                xt = xr.next()
                kb.dma(SP, xt.h[:], Xin[tile_idx * 128:(tile_idx + 1) * 128, :], w=[xt.d])
                tt = tr.next()
                kb.op(DVE, lambda tt=tt, ti=ti: nc_v.tensor_tensor(out=tt.h[:], in0=yacc.h[:, ti, :], in1=gate2.h[:],
                                                                   op=ALU.mult), r=[yd[ti], gate2.d], w=[tt.d])
                ot = orr.next()
                kb.op(POOL, lambda ot=ot, tt=tt, xt=xt: nc_g.tensor_tensor(out=ot.h[:], in0=tt.h[:], in1=xt.h[:], op=ALU.add),
                      r=[tt.d, xt.d], w=[ot.d])
                if out is None:
                    kb.dma(SP, Xout[tile_idx * 128:(tile_idx + 1) * 128, :], ot.h[:], r=[ot.d])
                else:
                    junk = junkr.next()
                    st = str_.next()
                    kb.op(ACT, lambda junk=junk, st=st, ot=ot: nc_s.activation(out=junk.h[:], in_=ot.h[:], func=AF.Square,
                                                                               scale=1.0 / 32.0, accum_out=st.h[:, 0:1]),
                          r=[ot.d], w=[junk.d, st.d])
                    kb.op(ACT, lambda st=st: nc_s.activation(out=st.h[:, 1:2], in_=st.h[:, 0:1], func=AF.Sqrt,
                                                             bias=epst.h[:, 0:1], scale=1.0), r=[epst.d], w=[st.d])
                    kb.op(DVE, lambda st=st: nc_v.reciprocal(out=st.h[:, 2:3], in_=st.h[:, 1:2]), w=[st.d])
                    o2 = tr.next()
                    kb.op(DVE, lambda o2=o2, ot=ot, st=st: nc_v.scalar_tensor_tensor(
                        out=o2.h[:], in0=ot.h[:], scalar=st.h[:, 2:3], in1=fg.h[:], op0=ALU.mult, op1=ALU.mult),
                          r=[ot.d, st.d, fg.d], w=[o2.d])
                    oi = tile_idx - 2
                    kb.dma(SP, out[oi * 128:(oi + 1) * 128, :], o2.h[:], r=[o2.d])


def _const_tables():
    t = {}
    t["c_ident"] = np.eye(128, dtype=np.float32)
    c = np.arange(128)
    ang = 2 * np.pi * np.outer(c, c) / 128.0
    t["c_fc"] = (np.concatenate([np.cos(ang), -np.sin(ang)], axis=1) / 1024.0).astype(np.float32)
    t["c_f128"] = np.stack([np.cos(ang), np.sin(ang), -np.sin(ang)]).astype(np.float32)
    k1 = np.arange(128)[:, None]
    l2 = np.arange(64)[None, :]
    a = 2 * np.pi * k1 * l2 / 8192.0
    t["c_tw"] = np.stack([np.cos(a), -np.sin(a)], axis=1).astype(np.float32)
    kk = np.arange(128)[:, None]
    qq = np.arange(128)[None, :]
    mp = (kk >= qq).astype(np.float32)
    mn = (kk <= qq).astype(np.float32)
    t["c_mask"] = np.stack([np.tile(mp, (1, 4)), np.tile(mn, (1, 4))]).astype(np.float32)
    return t


def _core_tables(s):
    t = {}
    g = 32 * s - 2 + np.arange(NT)
    valid = (g >= 0) & (g < 64)
    l2 = np.arange(64)[:, None]
    a = 2 * np.pi * l2 * g[None, :] / 64.0
    f64 = np.stack([np.cos(a), np.sin(a)]) * valid[None, None, :]
    t["c_f64"] = f64.astype(np.float32)
    tv = np.ones((128, NTK), np.float32)
    tv[:, :NT] = valid[None, :].astype(np.float32)
    t["c_tvalid"] = tv
    pos = (g[:, None] * 128 + np.arange(128)[None, :]).reshape(-1).astype(np.float64)
    pos = np.clip(pos, 0, SEQ - 1)
    row = np.floor(pos / 64.0)
    col = pos - row * 64.0
    inv = 10000.0 ** (-np.arange(16, dtype=np.float64) / 16.0)
    ang = np.stack([row[:, None] * inv[None, :], col[:, None] * inv[None, :]], axis=1)
    cos = np.tile(np.cos(ang)[:, None, :, :], (1, 8, 1, 1)).reshape(NT * 128, 256)
    sin = np.tile(np.sin(ang)[:, None, :, :], (1, 8, 1, 1)).reshape(NT * 128, 256)
    cos = np.concatenate([cos, np.ones((256, 256))], axis=0)
    sin = np.concatenate([sin, np.zeros((256, 256))], axis=0)
    t["c_cos"] = cos.astype(np.float32)
    t["c_sin"] = sin.astype(np.float32)
    return t


def make_in_maps(inputs, cores=None):
    f = lambda a: np.ascontiguousarray(np.asarray(a, dtype=np.float32))
    x = f(inputs["x"])
    c = f(inputs["c"])
    ctx = f(inputs["ctx"])
    c_ctx = f(inputs["c_ctx"])
    shared = {
        "ada_w": f(inputs["ada_w"]), "ada_b": f(inputs["ada_b"]),
        "norm_mix_g": f(inputs["norm_mix_g"]), "norm_ffn_g": f(inputs["norm_ffn_g"]),
        "final_norm_g": f(inputs["final_norm_g"]).reshape(1, D),
        "w_in": f(inputs["even_w_in"][0]), "w_in_fT": f(np.asarray(inputs["even_w_in"][0])[:, :512].T),
        "w_out": f(inputs["even_w_out"][0]), "sink": f(inputs["even_sink"]).reshape(1, 8),
        "pw1_w": f(inputs["conv_pw1_w"][0]), "pw1_b": f(inputs["conv_pw1_b"]).reshape(1, 2 * D),
        "dw_wT": f(np.asarray(inputs["conv_dw_w"][0]).T), "dw_b": f(inputs["conv_dw_b"]).reshape(1, D),
        "ln_g": f(inputs["conv_ln_g"]).reshape(1, D), "ln_b": f(inputs["conv_ln_b"]).reshape(1, D),
        "pw2_w": f(inputs["conv_pw2_w"][0]), "pw2_b": f(inputs["conv_pw2_b"]).reshape(1, D),
        "router_w": f(inputs["router_w"]), "router_b": f(inputs["router_b"]).reshape(1, NE),
        "moe_w_gate": f(inputs["moe_w_gate"]), "moe_w_up": f(inputs["moe_w_up"]), "moe_w_down": f(inputs["moe_w_down"]),
    }
    shared.update(_const_tables())
    ctabs = [_core_tables(0), _core_tables(1)]
    maps = []
    for core in (cores if cores is not None else range(8)):
        b, s = core // 2, core % 2
        m = dict(shared)
        m.update(ctabs[s])
        xp = np.zeros((NT * 128, D), np.float32)
        g0 = 32 * s - 2
        lo, hi = max(g0, 0), min(g0 + NT, 64)
        xp[(lo - g0) * 128:(hi - g0) * 128] = x[b, lo * 128:hi * 128]
        m["xw"] = xp
        m["xfull"] = x[b]
        m["ctxb"] = ctx[b]
        cv = np.stack([c[b], c_ctx], axis=0)
        m["cvecT"] = np.ascontiguousarray(cv.reshape(2, 8, 128).transpose(2, 1, 0))
        maps.append(m)
    return maps


_PROGRAM = None


def kernel(**inputs):
    global _PROGRAM
    if _PROGRAM is None:
        _PROGRAM = build_program()[0]
    maps = make_in_maps(inputs)
    res = run_bass_kernel_spmd(_PROGRAM, maps, core_ids=list(range(8)))
    outp = np.empty((4, SEQ, D), np.float32)
    for core in range(8):
        b, s = core // 2, core % 2
        outp[b, s * 4096:(s + 1) * 4096] = np.asarray(res.results[core]["out"]).reshape(4096, D)
    return outp
```
